# Optimizing a Trainium2 kernel written in Bass

```python
import math
import jax, jax.numpy as jnp
from jax import lax
import numpy as np

D_MODEL = 1024
BATCH = 8
SEQ = 4096
DEPTH = 1

MIX_WIDTH = D_MODEL
DIFF_WIDTH = MIX_WIDTH // 2
GMLP_WIDTH = MIX_WIDTH - DIFF_WIDTH
DIFF_HEADS = 4
DIFF_HEAD_DIM = DIFF_WIDTH // (2 * DIFF_HEADS)
QK_WIDTH = DIFF_HEADS * DIFF_HEAD_DIM
GMLP_GROUPS = 8
GMLP_GROUP_DIM = GMLP_WIDTH // GMLP_GROUPS
CHUNK = 128
Q_BLOCK = 128
IN_WIDTH = 4 * QK_WIDTH + DIFF_WIDTH + 2 * GMLP_WIDTH
N_EXPERTS = 32
TOP_K = 4
D_FF = D_MODEL
SWIGLU_LIMIT = 7.0
SWIGLU_ALPHA = 1.702
EXPERT_BLOCK = 128
LN_EPS = 1e-5
DEEPNORM_ALPHA = (2.0 * DEPTH) ** 0.25
DEEPNORM_BETA = (8.0 * DEPTH) ** -0.25

kernel_name = "hybrid_diffattn_gmlp_moe_deepnorm"


def layer_norm(x, g, b):
    xf = x.astype(jnp.float32)
    mu = jnp.mean(xf, axis=-1, keepdims=True)
    var = jnp.mean(jnp.square(xf - mu), axis=-1, keepdims=True)
    return ((xf - mu) * lax.rsqrt(var + LN_EPS)).astype(x.dtype) * g + b


def rms_norm(x, g):
    xf = x.astype(jnp.float32)
    ms = jnp.mean(jnp.square(xf), axis=-1, keepdims=True)
    return (xf * lax.rsqrt(ms + LN_EPS)).astype(x.dtype) * g


def diff_attention(q1, q2, k1, k2, v, lam):
    B, S, H, Dh = q1.shape
    nb = S // Q_BLOCK
    scale = Dh ** -0.5
    kpos = jnp.arange(S)
    lam32 = lam.astype(jnp.float32)

    def to_blocks(q):
        return q.reshape(B, nb, Q_BLOCK, H, Dh).transpose(1, 0, 2, 3, 4)

    def one_block(args):
        q1b, q2b, bi = args
        qpos = bi * Q_BLOCK + jnp.arange(Q_BLOCK)
        causal = kpos[None, :] <= qpos[:, None]

        def probs(qb, k):
            s = jnp.einsum('bqhd,bkhd->bhqk', qb, k).astype(jnp.float32) * scale
            return jax.nn.softmax(jnp.where(causal, s, -jnp.inf), axis=-1)

        a = probs(q1b, k1) - lam32 * probs(q2b, k2)
        return jnp.einsum('bhqk,bkhe->bqhe', a.astype(v.dtype), v)

    out = lax.map(one_block, (to_blocks(q1), to_blocks(q2), jnp.arange(nb)))
    return out.transpose(1, 0, 2, 3, 4).reshape(B, S, H, v.shape[-1])


def spatial_gating(gu, gv, ln_g, ln_b, w_s, b_s):
    B, S, _ = gu.shape
    u = jax.nn.gelu(gu, approximate=False)
    v = layer_norm(jax.nn.gelu(gv, approximate=False), ln_g, ln_b)
    vc = v.reshape(B, S // CHUNK, CHUNK, GMLP_GROUPS, GMLP_GROUP_DIM)
    causal = jnp.tril(jnp.ones((CHUNK, CHUNK), dtype=bool))
    w = jnp.where(causal[None], w_s, jnp.zeros_like(w_s))
    z = jnp.einsum('gts,bcsgd->bctgd', w, vc) + b_s.T[None, None, :, :, None]
    return u * z.reshape(B, S, GMLP_WIDTH)


def token_mixer(x, w_in, lq1, lk1, lq2, lk2, subln_g, gln_g, gln_b, w_s, b_s, w_o, lambda_init):
    B, S, _ = x.shape
    proj = x @ w_in
    cuts = [QK_WIDTH, 2 * QK_WIDTH, 3 * QK_WIDTH, 4 * QK_WIDTH,
            4 * QK_WIDTH + DIFF_WIDTH, 4 * QK_WIDTH + DIFF_WIDTH + GMLP_WIDTH]
    q1, q2, k1, k2, v, gu, gv = jnp.split(proj, cuts, axis=-1)
    heads = lambda t: t.reshape(B, S, DIFF_HEADS, -1)
    lam = jnp.exp(jnp.sum(lq1 * lk1)) - jnp.exp(jnp.sum(lq2 * lk2)) + lambda_init
    attn = diff_attention(heads(q1), heads(q2), heads(k1), heads(k2), heads(v), lam)
    attn = (rms_norm(attn, subln_g) * (1.0 - lambda_init)).reshape(B, S, DIFF_WIDTH)
    gated = spatial_gating(gu, gv, gln_g, gln_b, w_s, b_s)
    return jnp.concatenate([attn, gated], axis=-1) @ w_o


def moe_ffn(x, w_router, b_router, w_up, b_up, w_down, b_down):
    B, S, D = x.shape
    N = B * S
    A = N * TOP_K
    P = A + N_EXPERTS * EXPERT_BLOCK
    nblk = P // EXPERT_BLOCK
    xt = x.reshape(N, D)
    logits = xt @ w_router + b_router
    topv, topi = lax.top_k(logits, TOP_K)
    gates = jax.nn.softmax(topv.astype(jnp.float32), axis=-1).astype(x.dtype)

    flat_e = topi.reshape(A)
    flat_tok = jnp.repeat(jnp.arange(N, dtype=jnp.int32), TOP_K)
    flat_g = gates.reshape(A)
    order = jnp.argsort(flat_e, stable=True)
    sorted_e = flat_e[order]
    sorted_tok = flat_tok[order]
    sorted_g = flat_g[order]

    counts = jnp.bincount(flat_e, length=N_EXPERTS)
    padded = ((counts + EXPERT_BLOCK - 1) // EXPERT_BLOCK) * EXPERT_BLOCK
    start_sorted = jnp.cumsum(counts) - counts
    end_padded = jnp.cumsum(padded)
    start_padded = end_padded - padded
    rank = jnp.arange(A) - start_sorted[sorted_e]
    dest = start_padded[sorted_e] + rank

    tok_pad = jnp.zeros((P,), jnp.int32).at[dest].set(sorted_tok)
    x_blocks = xt[tok_pad].reshape(nblk, EXPERT_BLOCK, D)
    blk_start = jnp.arange(nblk) * EXPERT_BLOCK
    blk_expert = jnp.clip(jnp.searchsorted(end_padded, blk_start, side='right'), 0, N_EXPERTS - 1)

    def expert_block(args):
        xb, e = args
        h = xb @ w_up[e] + b_up[e]
        gate, lin = h[:, :D_FF], h[:, D_FF:]
        gate = jnp.minimum(gate, SWIGLU_LIMIT)
        lin = jnp.clip(lin, -SWIGLU_LIMIT, SWIGLU_LIMIT)
        act = (lin + 1.0) * gate * jax.nn.sigmoid(SWIGLU_ALPHA * gate)
        return act @ w_down[e] + b_down[e]

    y_pad = lax.map(expert_block, (x_blocks, blk_expert)).reshape(P, D)
    y_assign = y_pad[dest] * sorted_g[:, None]
    out = jax.ops.segment_sum(y_assign, sorted_tok, num_segments=N)
    return out.reshape(B, S, D)


def setup_inputs(seed: int = 0) -> dict:
    key = jax.random.key(seed)
    ks = jax.random.split(key, 24)
    L = DEPTH
    nrm = lambda k, shape, s: jax.random.normal(k, shape, jnp.float32) * s
    tril = jnp.tril(jnp.ones((CHUNK, CHUNK), jnp.float32))
    return {
        "x": nrm(ks[0], (BATCH, SEQ, D_MODEL), 1.0),
        "w_in": nrm(ks[1], (L, D_MODEL, IN_WIDTH), D_MODEL ** -0.5),
        "lambda_q1": nrm(ks[2], (L, DIFF_HEAD_DIM), 0.1),
        "lambda_k1": nrm(ks[3], (L, DIFF_HEAD_DIM), 0.1),
        "lambda_q2": nrm(ks[4], (L, DIFF_HEAD_DIM), 0.1),
        "lambda_k2": nrm(ks[5], (L, DIFF_HEAD_DIM), 0.1),
        "subln_g": 1.0 + nrm(ks[6], (L, 2 * DIFF_HEAD_DIM), 0.01),
        "gmlp_ln_g": 1.0 + nrm(ks[7], (L, GMLP_WIDTH), 0.01),
        "gmlp_ln_b": nrm(ks[8], (L, GMLP_WIDTH), 0.01),
        "w_spatial": nrm(ks[9], (L, GMLP_GROUPS, CHUNK, CHUNK), CHUNK ** -0.5) * tril,
        "b_spatial": 1.0 + nrm(ks[10], (L, GMLP_GROUPS, CHUNK), 0.01),
        "w_o": nrm(ks[11], (L, MIX_WIDTH, D_MODEL), MIX_WIDTH ** -0.5 * DEEPNORM_BETA),
        "ln1_g": 1.0 + nrm(ks[12], (L, D_MODEL), 0.01),
        "ln1_b": nrm(ks[13], (L, D_MODEL), 0.01),
        "w_router": nrm(ks[14], (L, D_MODEL, N_EXPERTS), D_MODEL ** -0.5),
        "b_router": nrm(ks[15], (L, N_EXPERTS), 0.01),
        "w_up": nrm(ks[16], (L, N_EXPERTS, D_MODEL, 2 * D_FF), D_MODEL ** -0.5),
        "b_up": nrm(ks[17], (L, N_EXPERTS, 2 * D_FF), 0.01),
        "w_down": nrm(ks[18], (L, N_EXPERTS, D_FF, D_MODEL), D_FF ** -0.5 * DEEPNORM_BETA),
        "b_down": nrm(ks[19], (L, N_EXPERTS, D_MODEL), 0.01),
        "ln2_g": 1.0 + nrm(ks[20], (L, D_MODEL), 0.01),
        "ln2_b": nrm(ks[21], (L, D_MODEL), 0.01),
    }


def reference(x, w_in, lambda_q1, lambda_k1, lambda_q2, lambda_k2, subln_g, gmlp_ln_g, gmlp_ln_b,
              w_spatial, b_spatial, w_o, ln1_g, ln1_b, w_router, b_router, w_up, b_up,
              w_down, b_down, ln2_g, ln2_b):
    for l in range(DEPTH):
        lambda_init = 0.8 - 0.6 * math.exp(-0.3 * l)
        mixed = token_mixer(x, w_in[l], lambda_q1[l], lambda_k1[l], lambda_q2[l], lambda_k2[l],
                            subln_g[l], gmlp_ln_g[l], gmlp_ln_b[l], w_spatial[l], b_spatial[l],
                            w_o[l], lambda_init)
        x = layer_norm(DEEPNORM_ALPHA * x + mixed, ln1_g[l], ln1_b[l])
        ffn = moe_ffn(x, w_router[l], b_router[l], w_up[l], b_up[l], w_down[l], b_down[l])
        x = layer_norm(DEEPNORM_ALPHA * x + ffn, ln2_g[l], ln2_b[l])
    return x
```

```python
import numpy as np
from contextlib import ExitStack
import concourse.bass as bass
import concourse.mybir as mybir
from concourse.bass_utils import run_bass_kernel_spmd

F32 = mybir.dt.float32
BF16 = mybir.dt.bfloat16
I32 = mybir.dt.int32
AF = mybir.ActivationFunctionType
ALU = mybir.AluOpType
AX = mybir.AxisListType

D = 1024
SEQ = 4096
NT = 32
NG = 8
E = 32
C = 1024
NB = C // 128
NSLOT = E * C
TABW = 16
ZROW = NSLOT
BIG = float(NSLOT + 128)
ALPHA = 2.0 ** 0.25
LAMBDA_INIT = 0.2
EPS = 1e-5
N_CORES = 8


class Buf:
    __slots__ = ("name", "w", "r")

    def __init__(self, name):
        self.name = name
        self.w = None
        self.r = []


class Sched:
    def __init__(self, nc, es):
        self.nc = nc
        self.es = es
        self.engs = {"pe": nc.tensor, "act": nc.scalar, "dve": nc.vector, "pool": nc.gpsimd, "sp": nc.sync}
        self.sems = {}
        self.cnt = {}
        self.waited = {k: {} for k in self.engs}
        self.issuer = {}
        self.rec = None
        self.ring = 0
        for k in self.engs:
            self.sems[k] = es.enter_context(nc.semaphore("c_" + k))
            self.cnt[k] = 0

    def _wait(self, eng, tok, out=None):
        if tok is None:
            return
        key, val = tok
        if key == eng and eng in ("pe", "sp"):
            return
        if self.waited[eng].get(key, 0) >= val:
            return
        self.waited[eng][key] = val
        if out is None:
            self.engs[eng].wait_ge(self.sems[key], val)
        else:
            out.append((key, val))

    def _deps(self, eng, reads, writes):
        need = {}
        toks = [b.w for b in reads] + [b.w for b in writes] + [t for b in writes for t in b.r]
        for t in toks:
            if t is not None and need.get(t[0], 0) < t[1]:
                need[t[0]] = t[1]
        out = []
        for k, v in need.items():
            self._wait(eng, (k, v), out)
        return out

    def _commit(self, tok, reads, writes):
        for b in reads:
            b.r.append(tok)
            if len(b.r) > 24:
                best = {}
                for (k, v) in b.r:
                    if best.get(k, 0) < v:
                        best[k] = v
                b.r = list(best.items())
        for b in writes:
            b.w = tok
            b.r = []

    def _emit(self, eng, waits, fn, sem, inc):
        def go():
            e = self.engs[eng]
            for (k, v) in waits:
                e.wait_ge(self.sems[k], v)
            ins = fn(e)
            if sem is not None:
                ins.then_inc(self.sems[sem], inc)
        if self.rec is not None:
            self.rec.append((eng, go))
        else:
            go()

    def op(self, eng, fn, reads=(), writes=(), sig=True):
        waits = self._deps(eng, reads, writes)
        if sig:
            self.cnt[eng] += 1
            tok = (eng, self.cnt[eng])
        else:
            tok = (eng, self.cnt[eng] + 1)
        self._emit(eng, waits, fn, eng if sig else None, 1)
        self._commit(tok, reads, writes)
        return tok

    def dma(self, eng, fn, sem, reads=(), writes=(), per=None):
        if per is not None:
            sem = f"{sem}_{per}"
        elif sem == "setup":
            if eng == "pool":
                sem = f"setup_pool{self.ring % 8}"
                self.ring += 1
            else:
                sem = f"setup_{eng}"
        if sem not in self.sems:
            self.sems[sem] = self.es.enter_context(self.nc.semaphore("d_" + sem))
            self.cnt[sem] = 0
        waits = self._deps(eng, reads, writes)
        self.issuer.setdefault(sem, set()).add(eng)
        self.cnt[sem] += 16
        tok = (sem, self.cnt[sem])
        self._emit(eng, waits, fn, sem, 16)
        self._commit(tok, reads, writes)
        return tok

    def cond_region(self, regs, bound, body):
        snap = dict(self.cnt)
        snap_waited = {k: dict(v) for k, v in self.waited.items()}
        assert self.rec is None
        self.rec = []
        body()
        rec, self.rec = self.rec, None
        delta = {k: self.cnt[k] - snap.get(k, 0) for k in self.cnt if self.cnt[k] != snap.get(k, 0)}
        assert all(k in self.engs or len(self.issuer[k]) == 1 for k in delta)
        for name, eng in self.engs.items():
            thunks = [go for (en, go) in rec if en == name]
            mine = [k for k in delta if k == name or self.issuer.get(k) == {name}]
            if not thunks and not mine:
                continue
            with eng.If_lt(regs[name], bound):
                for go in thunks:
                    go()
            with eng.Else():
                for k in mine:
                    if snap.get(k, 0) > 0:
                        eng.wait_ge(self.sems[k], snap.get(k, 0))
                    eng.sem_inc(self.sems[k], delta[k])
        self.waited = snap_waited

    def barrier(self):
        for eng in self.engs:
            for key in list(self.sems):
                if self.cnt[key] > 0:
                    self._wait(eng, (key, self.cnt[key]))


def build(debug=False):
    nc = bass.Bass("TRN2", target_bir_lowering=False)

    def din(name, shape, dtype=F32):
        return nc.dram_tensor(name, shape, dtype, kind="ExternalInput").ap()

    x = din("x", [SEQ, D])
    w_in = din("w_in", [D, 2560])
    lq1 = din("lambda_q1", [1, 64]); lk1 = din("lambda_k1", [1, 64])
    lq2 = din("lambda_q2", [1, 64]); lk2 = din("lambda_k2", [1, 64])
    subln_g = din("subln_g", [1, 128])
    gln_g = din("gmlp_ln_g", [1, 512]); gln_b = din("gmlp_ln_b", [1, 512])
    w_sp = din("w_spatial", [8, 128, 128]); b_sp = din("b_spatial", [8, 128])
    w_o = din("w_o", [D, D])
    ln1_g = din("ln1_g", [1, D]); ln1_b = din("ln1_b", [1, D])
    w_r = din("w_router", [D, E]); b_r = din("b_router", [1, E])
    w_up = din("w_up", [E, D, 2 * D]); b_up = din("b_up", [E, 2 * D])
    w_dn = din("w_down", [E, D, D]); b_dn = din("b_down", [E, D])
    ln2_g = din("ln2_g", [1, D]); ln2_b = din("ln2_b", [1, D])
    c_ident = din("c_ident", [128, 128])
    c_maskT = din("c_maskT", [128, 128])
    c_tri = din("c_tri", [128, 128])
    c_ones = din("c_ones", [128, 128])
    c_tril = din("c_tril", [128, 128])
    c_tokid = din("c_tokid", [128, NT, TABW], I32)
    c_ecb = din("c_ecb", [128, 4, E])

    out = nc.dram_tensor("out", [SEQ, D], F32, kind="ExternalOutput").ap()
    skind = "ExternalOutput" if debug else "Internal"
    x1f = nc.dram_tensor("x1f", [SEQ, D], F32, kind=skind).ap()
    x1b = nc.dram_tensor("x1b", [SEQ, D], BF16, kind=skind).ap()
    tab = nc.dram_tensor("tab", [NSLOT, TABW], I32, kind=skind).ap()
    y_all = nc.dram_tensor("y_all", [NSLOT + 1, D], BF16, kind=skind).ap()
    if debug:
        dbgA = nc.dram_tensor("dbgA", [NG, 128, 4, 512], BF16, kind="ExternalOutput").ap()
        dbgG = nc.dram_tensor("dbgG", [NG, 128, 4, 512], BF16, kind="ExternalOutput").ap()

    with ExitStack() as es:
        S = Sched(nc, es)
        banks = [es.enter_context(nc.psum_tensor(f"bk{i}", [128, 512], F32)) for i in range(8)]
        bbuf = [Buf(f"bk{i}") for i in range(8)]
        rot = {"list": [0, 1, 2, 3], "p": 0}

        def nb():
            i = rot["list"][rot["p"] % len(rot["list"])]
            rot["p"] += 1
            return banks[i], bbuf[i]

        def T(stack, name, shape, dtype):
            return stack.enter_context(nc.sbuf_tensor(name, shape, dtype)), Buf(name)

        def mm(out_ap, lhsT, rhs, start, stop, reads, writes, sig, sgc=False):
            return S.op("pe", lambda e: e.matmul(out_ap, lhsT, rhs, start=start, stop=stop, skip_group_check=sgc),
                        reads=reads, writes=writes, sig=sig)

        def tp(out_ap, in_ap, ident_ap, reads, writes, sig):
            return S.op("pe", lambda e: e.transpose(out_ap, in_ap, ident_ap), reads=reads, writes=writes, sig=sig)

        def bcast_load(dst, src, n, wbuf, eng="sp"):
            S.dma(eng, lambda e: e.dma_start(out=dst, in_=src.to_broadcast([128, n])), "setup", writes=[wbuf])

        ident, b_ident = T(es, "ident", [128, 128], BF16)
        ones, b_ones = T(es, "ones", [128, 128], BF16)
        onesf, b_onesf = T(es, "onesf", [33, 128], F32)
        tokid, b_tokid = T(es, "tokid", [128, NT, TABW], I32)
        zer, b_zer = T(es, "zer", [1, 512], BF16)
        big0 = es.enter_context(nc.sbuf_tensor("big0", [128, 24576], BF16))
        win = big0[:, 0:20480].rearrange("p (c n) -> p c n", c=8)
        xT = big0[:, 20480:24576].rearrange("p (c n) -> p c n", c=8)
        wup0 = big0[:, 0:16384].rearrange("p (c n) -> p c n", c=8)
        wdn0 = big0[:, 16384:24576].rearrange("p (c n) -> p c n", c=8)
        b_wup = [[Buf(f"wup{i}_{hh}") for hh in range(2)] for i in range(2)]
        b_wdn = [Buf("wdn0"), Buf("wdn1")]
        cntv, b_cntv = T(es, "cntv", [1, E], I32)
        S.op("pool", lambda e: e.memset(zer[:], 0.0), writes=[b_zer])
        slot4s, b_slot4s = T(es, "slot4s", [128, NT, 4], I32)
        slot4g, b_slot4g = T(es, "slot4g", [128, NT, 4], I32)
        gate4, b_gate4 = T(es, "gate4", [128, NT, 4], F32)
        bupT, b_bupT = T(es, "bupT", [128, 16, E], F32)
        S.dma("pool", lambda e: e.dma_start(out=ident[:], in_=c_ident), "setup", writes=[b_ident])
        S.dma("pool", lambda e: e.dma_start(out=ones[:], in_=c_ones), "setup", writes=[b_ones])
        S.dma("sp", lambda e: e.dma_start(out=onesf[:], in_=c_ones[0:33, :]), "setup", writes=[b_onesf])
        S.dma("sp", lambda e: e.dma_start(out=tokid[:], in_=c_tokid), "setup", writes=[b_tokid])

        with ExitStack() as p1:
            b_win = [Buf(f"win{j}") for j in range(5)]
            wo, b_wo = T(p1, "wo", [128, 8, D], BF16)
            wr, b_wr = T(p1, "wr", [128, 8, E], BF16)
            br, b_br = T(p1, "br", [128, E], F32)
            maskT, b_maskT = T(p1, "maskT", [128, 128], BF16)
            tri, b_tri = T(p1, "tri", [128, 128], BF16)
            ecb, b_ecb = T(p1, "ecb", [128, 4, E], F32)
            glng, b_glng = T(p1, "glng", [128, 512], F32)
            glnb, b_glnb = T(p1, "glnb", [128, 512], F32)
            ln1g, b_ln1g = T(p1, "ln1g", [128, D], F32)
            ln1b, b_ln1b = T(p1, "ln1b", [128, D], F32)
            g08c, b_g08c = T(p1, "g08c", [128, 1], F32)
            neglam, b_neglam = T(p1, "neglam", [128, 1], F32)
            WT, b_WT = T(p1, "WT", [128, 8, 128], BF16)
            bsT, b_bsT = T(p1, "bsT", [128, 8], F32)
            KT = p1.enter_context(nc.sbuf_tensor("KT", [128, 4, SEQ], BF16))
            b_KT = [[Buf(f"KT{c}_{g}") for g in range(NG)] for c in range(4)]
            Vsb = p1.enter_context(nc.sbuf_tensor("Vsb", [128, NT, 4, 129], BF16))
            b_V = [Buf(f"V{t}") for t in range(NT)]
            QT = p1.enter_context(nc.sbuf_tensor("QT", [128, 4, 512], BF16))
            b_QT = [Buf(f"QT{c}") for c in range(4)]
            b_xT = [Buf(f"xT{s}") for s in range(4)]
            GTs = [p1.enter_context(nc.sbuf_tensor(f"GT{i}", [128, 4, 512], BF16)) for i in range(2)]
            b_GTs = [[Buf(f"GT{i}_{s}") for s in range(4)] for i in range(2)]
            ATs = [p1.enter_context(nc.sbuf_tensor(f"AT{i}", [128, 4, 512], BF16)) for i in range(2)]
            b_ATs = [[Buf(f"AT{i}_{h}") for h in range(4)] for i in range(2)]
            p0 = ExitStack()
            identf, b_identf = T(p0, "identf", [128, 128], F32)
            S.dma("sp", lambda e: e.dma_start(out=identf[:], in_=c_ident), "setup", writes=[b_identf])
            lam_t, b_lam_t = T(p0, "lam_t", [128, 4, 64], F32)
            lam_s, b_lam_s = T(p0, "lam_s", [128, 2], F32)
            wsp, b_wsp = T(p0, "wsp", [128, 8, 128], F32)
            wspb, b_wspb = T(p0, "wspb", [128, 8, 128], BF16)
            trilt, b_trilt = T(p0, "trilt", [128, 128], F32)
            bsp8, b_bsp8 = T(p0, "bsp8", [8, 128], F32)
            bup_sb, b_bup_sb = T(p0, "bup_sb", [E, 2 * D], F32)
            ztab, b_ztab = T(p0, "ztab", [128, 64 * TABW], I32)
            zrow, b_zrow = T(p0, "zrow", [1, D], BF16)
            for j in range(2):
                for tt in range(2):
                    for hh in range(4):
                        S.dma("pool", lambda e, j=j, tt=tt, hh=hh: e.dma_start(
                            out=win[:, :, j * 512 + hh * 128 + tt * 64:j * 512 + hh * 128 + (tt + 1) * 64],
                            in_=w_in[:, j * 512 + tt * 256 + hh * 64:j * 512 + tt * 256 + (hh + 1) * 64].rearrange("(c p) d -> p c d", p=128)),
                            "setup", writes=[b_win[j]])
            for j in range(2, 5):
                S.dma("pool", lambda e, j=j: e.dma_start(
                    out=win[:, :, j * 512:(j + 1) * 512],
                    in_=w_in[:, j * 512:(j + 1) * 512].rearrange("(c p) n -> p c n", p=128)),
                    "setup", writes=[b_win[j]])
            S.dma("pool", lambda e: e.dma_start(out=maskT[:], in_=c_maskT), "setup", writes=[b_maskT])
            S.dma("pool", lambda e: e.dma_start(out=tri[:], in_=c_tri), "setup", writes=[b_tri])
            S.dma("sp", lambda e: e.dma_start(out=ecb[:], in_=c_ecb), "setup", writes=[b_ecb])
            S.dma("sp", lambda e: e.dma_start(out=trilt[:], in_=c_tril), "setup", writes=[b_trilt])
            S.dma("sp", lambda e: e.dma_start(out=wsp[:], in_=w_sp.rearrange("g t s -> t g s")), "setup", writes=[b_wsp])
            S.dma("sp", lambda e: e.dma_start(out=bsp8[:], in_=b_sp), "setup", writes=[b_bsp8])
            S.dma("sp", lambda e: e.dma_start(out=bup_sb[:], in_=b_up), "setup", writes=[b_bup_sb])
            bcast_load(glng[:], gln_g, 512, b_glng)
            bcast_load(glnb[:], gln_b, 512, b_glnb)
            bcast_load(ln1g[:], ln1_g, D, b_ln1g)
            bcast_load(ln1b[:], ln1_b, D, b_ln1b)
            with nc.allow_non_contiguous_dma(reason="128-element per-partition gain column"):
                S.dma("sp", lambda e: e.dma_start(out=g08c[:], in_=subln_g.rearrange("o n -> n o")), "setup", writes=[b_g08c])
            bcast_load(br[:], b_r, E, b_br)
            for i, src in enumerate([lq1, lk1, lq2, lk2]):
                bcast_load(lam_t[:, i, :], src, 64, b_lam_t)
            S.dma("pool", lambda e: e.dma_start(out=wo[:], in_=w_o.rearrange("(c p) n -> p c n", p=128)),
                  "setup", writes=[b_wo])
            S.dma("pool", lambda e: e.dma_start(out=wr[:], in_=w_r.rearrange("(c p) n -> p c n", p=128)),
                  "setup", writes=[b_wr])

            S.barrier()
            S.op("pool", lambda e: e.memset(ztab[:], 0), writes=[b_ztab])
            S.op("pool", lambda e: e.memset(zrow[:], 0.0), writes=[b_zrow])
            b_tab = Buf("tab")
            for q in range(NSLOT // (128 * 64)):
                S.dma("sp", lambda e, q=q: e.dma_start(out=tab[q * 8192:(q + 1) * 8192, :].rearrange("(p c) w -> p (c w)", p=128), in_=ztab[:]),
                      "setup", reads=[b_ztab], writes=[b_tab], per="ztab")
            S.dma("sp", lambda e: e.dma_start(out=y_all[ZROW:ZROW + 1, :], in_=zrow[:]), "setup", reads=[b_zrow], per="zrow")

            S.op("dve", lambda e: e.tensor_tensor(out=lam_t[:, 0, :], in0=lam_t[:, 0, :], in1=lam_t[:, 1, :], op=ALU.mult),
                 reads=[b_lam_t], writes=[b_lam_t])
            S.op("dve", lambda e: e.tensor_tensor(out=lam_t[:, 2, :], in0=lam_t[:, 2, :], in1=lam_t[:, 3, :], op=ALU.mult),
                 reads=[b_lam_t], writes=[b_lam_t])
            S.op("dve", lambda e: e.tensor_reduce(out=lam_s[:, 0:1], in_=lam_t[:, 0, :], axis=AX.X, op=ALU.add),
                 reads=[b_lam_t], writes=[b_lam_s])
            S.op("dve", lambda e: e.tensor_reduce(out=lam_s[:, 1:2], in_=lam_t[:, 2, :], axis=AX.X, op=ALU.add),
                 reads=[b_lam_t], writes=[b_lam_s])
            S.op("act", lambda e: e.activation(lam_s[:], lam_s[:], AF.Exp), reads=[b_lam_s], writes=[b_lam_s])
            S.op("dve", lambda e: e.scalar_tensor_tensor(out=neglam[:], in0=lam_s[:, 1:2], scalar=-LAMBDA_INIT,
                                                         in1=lam_s[:, 0:1], op0=ALU.add, op1=ALU.subtract),
                 reads=[b_lam_s], writes=[b_neglam])
            S.op("dve", lambda e: e.tensor_scalar(out=g08c[:], in0=g08c[:], scalar1=1.0 - LAMBDA_INIT, scalar2=None, op0=ALU.mult),
                 reads=[b_g08c], writes=[b_g08c])
            for gq in range(8):
                S.op("dve", lambda e, gq=gq: e.tensor_tensor(out=wspb[:, gq, :], in0=wsp[:, gq, :], in1=trilt[:], op=ALU.mult),
                     reads=[b_wsp, b_trilt], writes=[b_wspb])
            bk, bb = nb()
            bkb = bk[:].bitcast(BF16)
            for gq in range(8):
                tp(bkb[:, gq * 128:(gq + 1) * 128], wspb[:, gq, :], ident[:], [b_wspb, b_ident], [bb], gq == 7)
            S.op("dve", lambda e: e.tensor_copy(WT[:], bkb.rearrange("p (g t) -> p g t", g=8)), reads=[bb], writes=[b_WT])
            bk, bb = nb()
            tp(bk[:, 0:8], bsp8[:, :], identf[0:8, 0:8], [b_bsp8, b_identf], [bb], True)
            S.op("dve", lambda e: e.tensor_copy(bsT[:], bk[:, 0:8]), reads=[bb], writes=[b_bsT])
            bk, bb = nb()
            for m in range(16):
                tp(bk[:, m * E:(m + 1) * E], bup_sb[:, m * 128:(m + 1) * 128], identf[0:E, 0:E], [b_bup_sb, b_identf], [bb], m == 15)
            S.op("dve", lambda e: e.tensor_copy(bupT[:], bk[:].rearrange("p (m e) -> p m e", m=16)), reads=[bb], writes=[b_bupT])
            S.op("dve", lambda e: e.tensor_scalar(out=bupT[:, 8:16, :], in0=bupT[:, 8:16, :], scalar1=1.0 / 1.702, scalar2=None, op0=ALU.mult),
                 reads=[b_bupT], writes=[b_bupT])

            S.barrier()
            p0.close()
            xb = [T(p1, f"xb{i}", [128, D], BF16) for i in range(1)]
            u_sb = [T(p1, f"u{i}", [128, 512], BF16) for i in range(4)]
            gvy = p1.enter_context(nc.sbuf_tensor("gvy", [128, 4, 512], F32))
            b_gvy = [Buf(f"gvy{i}") for i in range(4)]
            gv_sb = [(gvy[:, i, :], b_gvy[i]) for i in range(4)]
            yb = [(gvy[:, 2 * i:2 * i + 2, :].rearrange("p a b -> p (a b)"), [b_gvy[2 * i], b_gvy[2 * i + 1]]) for i in range(2)]
            st1 = [T(p1, f"st1_{i}", [128, 6], F32) for i in range(4)]
            mvg, b_mvg = T(p1, "mvg", [128, 4, 2], F32)
            rstdg, b_rstdg = T(p1, "rstdg", [128, 4], F32)
            vbf, b_vbf = T(p1, "vbf", [128, 512], BF16)
            gated, b_gated = T(p1, "gated", [128, 512], BF16)
            pT = [[T(p1, f"pT{t}{k}", [128, 512], BF16) for k in range(2)] for t in range(2)]
            OTs = [p1.enter_context(nc.sbuf_tensor(f"OTs{i}", [128, 512], F32)) for i in range(2)]
            b_OTs = [Buf("OTs0"), Buf("OTs1")]
            accs_f, b_accs = OTs[0], b_OTs[0]
            Lsb = p1.enter_context(nc.sbuf_tensor("Lsb", [33, 512], F32))
            b_Lsb = [Buf("Lsb0"), Buf("Lsb1")]
            b_L = [Buf("L0"), Buf("L1")]
            sqb, b_sqb = T(p1, "sqb", [128, 512], BF16)
            st2 = [T(p1, f"st2_{i}", [128, 2, 6], F32) for i in range(4)]
            mv1, b_mv1 = T(p1, "mv1", [128, 4, 2], F32)
            rstd1, b_rstd1 = T(p1, "rstd1", [128, 4], F32)
            x1bf, b_x1bf = T(p1, "x1bf", [128, D], BF16)
            x1T, b_x1T = T(p1, "x1T", [128, 8, 128], BF16)
            lg, b_lg = T(p1, "lg", [128, 4, E], F32)
            m8, b_m8 = T(p1, "m8", [128, 4, 8], F32)
            msk, b_msk = T(p1, "msk", [128, 4, E], F32)
            mskb, b_mskb = T(p1, "mskb", [128, 4, E], BF16)
            ex, b_ex = T(p1, "ex", [128, 4, E], F32)
            gts, b_gts = T(p1, "gts", [128, 4, E], F32)
            pos, b_pos = T(p1, "pos", [128, 4, E], F32)
            okf, b_okf = T(p1, "okf", [128, 4, E], F32)
            sls, b_sls = T(p1, "sls", [128, 4, E], F32)
            slg, b_slg = T(p1, "slg", [128, 4, E], F32)
            oh, b_oh = msk, b_msk
            tmp, b_tmp = ex, b_ex
            base, b_base = T(p1, "base", [128, E], F32)
            sm4, b_sm4 = T(p1, "sm4", [128, 4], F32)
            f4s, b_f4s = T(p1, "f4s", [128, 4, 4], F32)
            f4g, b_f4g = T(p1, "f4g", [128, 4, 4], F32)

            acc_bufs = [Buf(f"acc{a}") for a in range(8)]
            breg = nc.gpsimd.to_reg(NSLOT - 1)
            S.op("pool", lambda e: e.memset(base[:], 0.0), writes=[b_base])

            def acc_ap(a, lo, hi):
                bank = banks[4 + a // 3]
                off = (a % 3) * 129
                return bank[:, off + lo:off + hi]

            pcount = [0, 0]
            b_x1f_d = Buf("x1f_d")
            b_x1b_d = Buf("x1b_d")

            def proj(g):
                for s in range(4):
                    t = 4 * g + s
                    xbt, b_xbt = xb[0]
                    S.dma("pool", lambda e, t=t, xbt=xbt: e.dma_start(out=xbt[:], in_=x[t * 128:(t + 1) * 128, :]),
                          "xld", writes=[b_xbt])
                    bk, bb = nb()
                    bkb = bk[:].bitcast(BF16)
                    for c in range(8):
                        tp(bkb[:, c * 128:(c + 1) * 128], xbt[:, c * 128:(c + 1) * 128], ident[:], [b_xbt, b_ident], [bb], c == 7)
                    S.op("dve", lambda e, s=s, bkb=bkb: e.tensor_copy(xT[:, :, s * 128:(s + 1) * 128],
                                                                     bkb.rearrange("p (c t) -> p c t", c=8)),
                         reads=[bb], writes=[b_xT[s]])
                for s in range(4):
                    t = 4 * g + s
                    bk, bb = nb()
                    for c in range(8):
                        mm(bk[:, :], xT[:, c, s * 128:(s + 1) * 128], win[:, c, 1024:1536], c == 0, c == 7,
                           [b_win[2], b_xT[s]], [bb], c == 7)
                    S.op("dve", lambda e, t=t, bk=bk: e.tensor_copy(Vsb[:, t, :, 0:128], bk[:].rearrange("p (h d) -> p h d", h=4)),
                         reads=[bb], writes=[b_V[t]])
                    bk, bb = nb()
                    for c in range(8):
                        mm(bk[:, :], xT[:, c, s * 128:(s + 1) * 128], win[:, c, 1536:2048], c == 0, c == 7,
                           [b_win[3], b_xT[s]], [bb], c == 7)
                    ut, b_ut = u_sb[s]
                    S.op("act", lambda e, ut=ut, bk=bk: e.activation(ut[:], bk[:, :], AF.Gelu), reads=[bb], writes=[b_ut])
                    bk, bb = nb()
                    for c in range(8):
                        mm(bk[:, :], xT[:, c, s * 128:(s + 1) * 128], win[:, c, 2048:2560], c == 0, c == 7,
                           [b_win[4], b_xT[s]], [bb], c == 7)
                    gvt, b_gvt = gv_sb[s]
                    S.op("act", lambda e, gvt=gvt, bk=bk: e.activation(gvt, bk[:, :], AF.Gelu), reads=[bb], writes=[b_gvt])
                    stt, b_stt = st1[s]
                    S.op("dve", lambda e, stt=stt, gvt=gvt: e.bn_stats(stt[:], gvt), reads=[b_gvt], writes=[b_stt])
                    S.op("dve", lambda e, s=s, stt=stt: e.bn_aggr(mvg[:, s, :], stt[:]), reads=[b_stt], writes=[b_mvg])
                S.op("act", lambda e: e.activation(rstdg[:], mvg[:, :, 1], AF.Ln, bias=EPS, scale=1.0), reads=[b_mvg], writes=[b_rstdg])
                S.op("act", lambda e: e.activation(rstdg[:], rstdg[:], AF.Exp, scale=-0.5), reads=[b_rstdg], writes=[b_rstdg])

                def fm(m):
                    bk, bb = nb()
                    for c in range(8):
                        mm(bk[:, :], win[:, c, m * 128:(m + 1) * 128], xT[:, c, :], c == 0, c == 7,
                           [b_win[m // 4]] + b_xT, [bb], c == 7)
                    if m < 4:
                        dst, wb, sc = QT[:, m, :], b_QT[m], 0.125
                    else:
                        dst, wb, sc = KT[:, m - 4, g * 512:(g + 1) * 512], b_KT[m - 4][g], 1.0
                    if m % 2 == 0:
                        S.op("act", lambda e: e.activation(dst, bk[:, :], AF.Copy, scale=sc), reads=[bb], writes=[wb])
                    else:
                        S.op("dve", lambda e: e.tensor_scalar(out=dst, in0=bk[:, :], scalar1=sc, scalar2=None, op0=ALU.mult), reads=[bb], writes=[wb])

                def g1(s):
                    gvt, b_gvt = gv_sb[s]
                    S.op("dve", lambda e: e.tensor_scalar(out=gvt, in0=gvt, scalar1=mvg[:, s, 0:1], scalar2=rstdg[:, s:s + 1],
                                                          op0=ALU.subtract, op1=ALU.mult), reads=[b_gvt, b_mvg, b_rstdg], writes=[b_gvt])
                    S.op("pool", lambda e: e.tensor_tensor(out=gvt, in0=gvt, in1=glng[:], op=ALU.mult), reads=[b_gvt, b_glng], writes=[b_gvt])
                    S.op("pool", lambda e: e.tensor_tensor(out=vbf[:], in0=gvt, in1=glnb[:], op=ALU.add), reads=[b_gvt, b_glnb], writes=[b_vbf])

                def g2(s):
                    ut, b_ut = u_sb[s]
                    bk, bb = nb()
                    for gq in range(8):
                        mm(bk[:, gq * 64:(gq + 1) * 64], WT[:, gq, :], vbf[:, gq * 64:(gq + 1) * 64], True, True,
                           [b_WT, b_vbf], [bb], gq == 7)
                    S.op("dve", lambda e: e.tensor_tensor(out=accs_f[:].rearrange("p (g d) -> p g d", g=8),
                                                          in0=bk[:].rearrange("p (g d) -> p g d", g=8),
                                                          in1=bsT[:].unsqueeze(2).to_broadcast([128, 8, 64]), op=ALU.add),
                         reads=[bb, b_bsT], writes=[b_accs])
                    S.op("dve", lambda e: e.tensor_tensor(out=gated[:], in0=accs_f[:], in1=ut[:], op=ALU.mult),
                         reads=[b_accs, b_ut], writes=[b_gated])

                def g3(s):
                    bk, bb = nb()
                    bkb = bk[:].bitcast(BF16)
                    for c in range(4):
                        tp(bkb[:, c * 128:(c + 1) * 128], gated[:, c * 128:(c + 1) * 128], ident[:], [b_gated, b_ident], [bb], c == 3)
                    S.op("dve", lambda e: e.tensor_copy(GTs[g % 2][:, :, s * 128:(s + 1) * 128],
                                                        bkb[:, 0:512].rearrange("p (c t) -> p c t", c=4)),
                         reads=[bb], writes=[b_GTs[g % 2][s]])

                for step in (g1, 0), (fm, 0), (fm, 1), (g2, 0), (g1, 1), (fm, 2), (fm, 3), (g3, 0), (g2, 1), (g1, 2), \
                        (fm, 4), (fm, 5), (g3, 1), (g2, 2), (g1, 3), (fm, 6), (fm, 7), (g3, 2), (g2, 3), (g3, 3):
                    step[0](step[1])

            def attn(g, inject=None):
                nj = 4 * g + 4
                stages = []

                def fin_evac(h):
                    S.op("dve", lambda e: e.tensor_copy(OTs[0][:], banks[4][:, :]), reads=[bbuf[4]], writes=[b_OTs[0]])
                    S.op("dve", lambda e: e.tensor_copy(OTs[1][:], banks[5][:, :]), reads=[bbuf[5]], writes=[b_OTs[1]])
                    for r0, k in ((0, 0), (32, 1)):
                        S.op("act", lambda e, r0=r0: e.activation(Lsb[r0:r0 + 1, :], banks[6][r0:r0 + 1, :], AF.Ln), reads=[b_L[k]], writes=[b_Lsb[k]])
                        S.op("act", lambda e, r0=r0: e.activation(Lsb[r0:r0 + 1, :], Lsb[r0:r0 + 1, :], AF.Exp, scale=-1.0), reads=[b_Lsb[k]], writes=[b_Lsb[k]])

                def fin_a1(h):
                    mm(banks[7][:, :], onesf[0:1, :], Lsb[0:1, :], True, True, [b_onesf, b_Lsb[0]], [bbuf[7]], True)
                    S.op("dve", lambda e: e.tensor_tensor(out=OTs[0][:], in0=OTs[0][:], in1=banks[7][:, :], op=ALU.mult),
                         reads=[b_OTs[0], bbuf[7]], writes=[b_OTs[0]])

                def fin_a2(h):
                    mm(banks[7][:, :], onesf[32:33, :], Lsb[32:33, :], True, True, [b_onesf, b_Lsb[1]], [bbuf[7]], True)
                    S.op("dve", lambda e: e.scalar_tensor_tensor(out=OTs[1][:], in0=OTs[1][:], scalar=neglam[:, 0:1], in1=banks[7][:, :],
                                                                 op0=ALU.mult, op1=ALU.mult),
                         reads=[b_OTs[1], bbuf[7], b_neglam], writes=[b_OTs[1]])
                    S.op("dve", lambda e: e.tensor_tensor(out=OTs[0][:], in0=OTs[0][:], in1=OTs[1][:], op=ALU.add),
                         reads=[b_OTs[0], b_OTs[1]], writes=[b_OTs[0]])
                    S.op("pool", lambda e: e.tensor_tensor(out=sqb[:], in0=OTs[0][:], in1=OTs[0][:], op=ALU.mult), reads=[b_OTs[0]], writes=[b_sqb])

                def fin_b(h):
                    mm(banks[7][:, :], ones[:, :], sqb[:], True, True, [b_ones, b_sqb], [bbuf[7]], True)
                    S.op("act", lambda e: e.activation(OTs[1][:], banks[7][:, :], AF.Ln, bias=EPS, scale=1.0 / 128.0), reads=[bbuf[7]], writes=[b_OTs[1]])
                    S.op("act", lambda e: e.activation(OTs[1][:], OTs[1][:], AF.Exp, scale=-0.5), reads=[b_OTs[1]], writes=[b_OTs[1]])
                    S.op("dve", lambda e: e.scalar_tensor_tensor(out=ATs[g % 2][:, h, :], in0=OTs[0][:], scalar=g08c[:, 0:1], in1=OTs[1][:],
                                                                 op0=ALU.mult, op1=ALU.mult),
                         reads=[b_OTs[0], b_OTs[1], b_g08c], writes=[b_ATs[g % 2][h]])

                for h in range(4):
                    if inject is not None:
                        stages.extend(inject(h))

                    def issue_S(j):
                        dj = j - 4 * g
                        i0 = max(dj, 0)
                        c0 = i0 * 128
                        res = []
                        for typ in range(2):
                            bi_s = 2 * (j % 2) + typ
                            bk, bb = banks[bi_s], bbuf[bi_s]
                            r0 = 64 * typ
                            mm(bk[:, c0:512], KT[r0:r0 + 64, h, j * 128:(j + 1) * 128], QT[r0:r0 + 64, h, c0:512], True, True,
                               [b_KT[h][j // 4], b_QT[h]], [bb], typ == 1)
                            res.append((bk, bb))
                        return dj, i0, c0, res

                    cur = issue_S(0)
                    for j in range(nj):
                        nxt = issue_S(j + 1) if j + 1 < nj else None
                        dj, i0, c0, res = cur
                        ps = []
                        for typ in range(2):
                            bk, bb = res[typ]
                            p, bp = pT[typ][pcount[typ] % 2]
                            pcount[typ] += 1
                            S.op("act", lambda e: e.activation(p[:, c0:512], bk[:, c0:512], AF.Exp), reads=[bb], writes=[bp])
                            if dj >= 0:
                                S.op("dve", lambda e: e.tensor_tensor(out=p[:, c0:c0 + 128], in0=p[:, c0:c0 + 128], in1=maskT[:], op=ALU.mult),
                                     reads=[bp, b_maskT], writes=[bp])
                            ps.append((p, bp))
                        allp = [ps[0][1], ps[1][1]]
                        for typ in range(2):
                            p, bp = ps[typ]
                            mm(banks[4 + typ][:, c0:512], Vsb[:, j, h, 0:128], p[:, c0:512], j == 0, j == nj - 1,
                               allp + [b_V[j]], [bbuf[4 + typ]], False)
                        for typ in range(2):
                            p, bp = ps[typ]
                            S.op("pe", lambda e: e.matmul(banks[6][32 * typ:32 * typ + 1, c0:512], ones[:, 0:1], p[:, c0:512],
                                                          start=(j == 0), stop=(j == nj - 1), tile_position=(0, 32 * typ)),
                                 reads=allp + [b_ones], writes=[b_L[typ]], sig=(typ == 1))
                        cur = nxt
                        for (jj, fn, hh) in list(stages):
                            if j == min(jj, nj - 1):
                                fn(hh)
                                stages.remove((jj, fn, hh))
                    fin_evac(h)
                    stages.extend([(1, fin_a1, h), (2, fin_a2, h), (4, fin_b, h)])
                for (jj, fn, hh) in stages:
                    fn(hh)

                if debug:
                    S.dma("sp", lambda e, g=g: e.dma_start(out=dbgA[g], in_=ATs[g % 2][:]), "dbg", reads=b_ATs[g % 2], per=f"A{g % 2}")
                    S.dma("sp", lambda e, g=g: e.dma_start(out=dbgG[g], in_=GTs[g % 2][:]), "dbg", reads=b_GTs[g % 2], per=f"G{g % 2}")
            def wo_stages(g, s, bank=None):
                t = 4 * g + s
                par = g % 2
                yt, byt = yb[s % 2]
                stt, b_stt = st2[s]

                def getbank():
                    return (banks[bank], bbuf[bank]) if bank is not None else nb()

                def half_stage(half):
                    def f(_):
                        if half == 0:
                            S.dma("sp", lambda e: e.dma_start(out=yt, in_=x[t * 128:(t + 1) * 128, :]), "xfld", writes=byt, per=s % 2)
                        bk, bb = getbank()
                        for c in range(8):
                            if c < 4:
                                lhs, rb = ATs[par][:, c, s * 128:(s + 1) * 128], b_ATs[par][c]
                            else:
                                lhs, rb = GTs[par][:, c - 4, s * 128:(s + 1) * 128], b_GTs[par][s]
                            mm(bk[:, :], lhs, wo[:, c, half * 512:(half + 1) * 512], c == 0, c == 7, [rb, b_wo], [bb], c == 7)
                        S.op("dve", lambda e: e.scalar_tensor_tensor(
                            out=yt[:, half * 512:(half + 1) * 512], in0=yt[:, half * 512:(half + 1) * 512], scalar=ALPHA, in1=bk[:, :],
                            op0=ALU.mult, op1=ALU.add), reads=byt + [bb], writes=[byt[half]])
                        S.op("dve", lambda e: e.bn_stats(stt[:, half, :], yt[:, half * 512:(half + 1) * 512]), reads=[byt[half]], writes=[b_stt])
                        if half == 1:
                            S.op("dve", lambda e: e.bn_aggr(mv1[:, s, :], stt[:]), reads=[b_stt], writes=[b_mv1])
                    return f

                def rstd_stage(_):
                    S.op("act", lambda e: e.activation(rstd1[:, s:s + 1], mv1[:, s, 1:2], AF.Ln, bias=EPS, scale=1.0), reads=[b_mv1], writes=[b_rstd1])
                    S.op("act", lambda e: e.activation(rstd1[:, s:s + 1], rstd1[:, s:s + 1], AF.Exp, scale=-0.5), reads=[b_rstd1], writes=[b_rstd1])

                def norm_stage(_):
                    S.op("dve", lambda e: e.tensor_scalar(out=yt, in0=yt, scalar1=mv1[:, s, 0:1], scalar2=rstd1[:, s:s + 1],
                                                          op0=ALU.subtract, op1=ALU.mult), reads=byt + [b_mv1, b_rstd1], writes=byt)
                    S.op("dve", lambda e: e.tensor_tensor(out=yt, in0=yt, in1=ln1g[:], op=ALU.mult), reads=byt + [b_ln1g], writes=byt)
                    S.op("dve", lambda e: e.tensor_tensor(out=yt, in0=yt, in1=ln1b[:], op=ALU.add), reads=byt + [b_ln1b], writes=byt)
                    S.op("dve", lambda e: e.tensor_copy(x1bf[:], yt), reads=byt, writes=[b_x1bf])
                    S.dma("sp", lambda e: e.dma_start(out=x1f[t * 128:(t + 1) * 128, :], in_=yt), "x1st", reads=byt, writes=[b_x1f_d], per=f"f{s % 2}")
                    S.dma("sp", lambda e: e.dma_start(out=x1b[t * 128:(t + 1) * 128, :], in_=x1bf[:]), "x1st", reads=[b_x1bf], writes=[b_x1b_d], per="b")

                def tr_stage(_):
                    bk, bb = getbank()
                    bkb = bk[:].bitcast(BF16)
                    for c in range(8):
                        tp(bkb[:, c * 128:(c + 1) * 128], x1bf[:, c * 128:(c + 1) * 128], ident[:], [b_x1bf, b_ident], [bb], c == 7)
                    S.op("dve", lambda e: e.tensor_copy(x1T[:], bkb.rearrange("p (c t) -> p c t", c=8)), reads=[bb], writes=[b_x1T])

                def router_stage(_):
                    bk, bb = getbank()
                    for c in range(8):
                        mm(bk[:, 0:E], x1T[:, c, :], wr[:, c, :], c == 0, c == 7, [b_x1T, b_wr], [bb], c == 7)
                    S.op("dve", lambda e: e.tensor_tensor(out=lg[:, s, :], in0=bk[:, 0:E], in1=br[:], op=ALU.add), reads=[bb, b_br], writes=[b_lg])
                    S.op("dve", lambda e: e.max(out=m8[:, s, :], in_=lg[:, s, :]), reads=[b_lg], writes=[b_m8])

                return [half_stage(0), half_stage(1), rstd_stage, norm_stage, tr_stage, router_stage]

            WO_J = (5, 6, 8, 9, 11, 13)

            def wo_ln(g):
                for s in range(4):
                    for f in wo_stages(g, s):
                        f(None)

            def routing(g):
                def bc(ap3):
                    return ap3.to_broadcast([128, 4, E])
                S.op("dve", lambda e: e.tensor_tensor(out=msk[:], in0=lg[:], in1=bc(m8[:, :, 3:4]), op=ALU.is_ge), reads=[b_lg, b_m8], writes=[b_msk])
                S.op("dve", lambda e: e.tensor_tensor(out=ex[:], in0=lg[:], in1=bc(m8[:, :, 0:1]), op=ALU.subtract), reads=[b_lg, b_m8], writes=[b_ex])
                S.op("act", lambda e: e.activation(ex[:], ex[:], AF.Exp), reads=[b_ex], writes=[b_ex])
                S.op("dve", lambda e: e.tensor_tensor(out=ex[:], in0=ex[:], in1=msk[:], op=ALU.mult), reads=[b_ex, b_msk], writes=[b_ex])
                S.op("dve", lambda e: e.tensor_reduce(out=sm4[:], in_=ex[:], axis=AX.X, op=ALU.add), reads=[b_ex], writes=[b_sm4])
                S.op("dve", lambda e: e.reciprocal(sm4[:], sm4[:]), reads=[b_sm4], writes=[b_sm4])
                S.op("dve", lambda e: e.tensor_tensor(out=gts[:], in0=ex[:], in1=bc(sm4[:].unsqueeze(2)), op=ALU.mult), reads=[b_ex, b_sm4], writes=[b_gts])
                S.op("dve", lambda e: e.tensor_copy(mskb[:], msk[:]), reads=[b_msk], writes=[b_mskb])
                bk, bb = nb()
                for s in range(4):
                    mm(bk[:, s * E:(s + 1) * E], tri[:], mskb[:, s, :], True, True, [b_tri, b_mskb], [bb], False)
                for s in range(4):
                    mm(bk[:, 128 + s * E:128 + (s + 1) * E], ones[:], mskb[:, s, :], True, True, [b_ones, b_mskb], [bb], s == 3)
                for s in range(4):
                    S.op("dve", lambda e, s=s, bk=bk: e.tensor_tensor(out=pos[:, s, :], in0=bk[:, s * E:(s + 1) * E], in1=base[:], op=ALU.add),
                         reads=[bb, b_base], writes=[b_pos])
                    S.op("dve", lambda e, s=s, bk=bk: e.tensor_tensor(out=base[:], in0=bk[:, 128 + s * E:128 + (s + 1) * E], in1=base[:], op=ALU.add),
                         reads=[bb, b_base], writes=[b_base])
                S.op("dve", lambda e: e.tensor_scalar(out=okf[:], in0=pos[:], scalar1=float(C), scalar2=None, op0=ALU.is_lt), reads=[b_pos], writes=[b_okf])
                S.op("dve", lambda e: e.tensor_tensor(out=pos[:], in0=pos[:], in1=ecb[:], op=ALU.add), reads=[b_pos, b_ecb], writes=[b_pos])
                S.op("dve", lambda e: e.tensor_scalar(out=sls[:], in0=pos[:], scalar1=-BIG, scalar2=None, op0=ALU.add), reads=[b_pos], writes=[b_sls])
                S.op("dve", lambda e: e.tensor_tensor(out=sls[:], in0=sls[:], in1=okf[:], op=ALU.mult), reads=[b_sls, b_okf], writes=[b_sls])
                S.op("dve", lambda e: e.tensor_scalar(out=sls[:], in0=sls[:], scalar1=BIG, scalar2=None, op0=ALU.add), reads=[b_sls], writes=[b_sls])
                S.op("dve", lambda e: e.tensor_scalar(out=slg[:], in0=pos[:], scalar1=-float(ZROW), scalar2=None, op0=ALU.add), reads=[b_pos], writes=[b_slg])
                S.op("dve", lambda e: e.tensor_tensor(out=slg[:], in0=slg[:], in1=okf[:], op=ALU.mult), reads=[b_slg, b_okf], writes=[b_slg])
                S.op("dve", lambda e: e.tensor_scalar(out=slg[:], in0=slg[:], scalar1=float(ZROW), scalar2=None, op0=ALU.add), reads=[b_slg], writes=[b_slg])
                for j in range(4):
                    S.op("dve", lambda e, j=j: e.tensor_tensor(out=oh[:], in0=lg[:], in1=bc(m8[:, :, j:j + 1]), op=ALU.is_equal),
                         reads=[b_lg, b_m8], writes=[b_oh])
                    for (src, bsrc, dst, bdst) in ((sls, b_sls, f4s, b_f4s), (slg, b_slg, f4g, b_f4g)):
                        S.op("dve", lambda e, src=src: e.tensor_tensor(out=tmp[:], in0=oh[:], in1=src[:], op=ALU.mult), reads=[b_oh, bsrc], writes=[b_tmp])
                        S.op("dve", lambda e, dst=dst, j=j: e.tensor_reduce(out=dst[:, :, j], in_=tmp[:], axis=AX.X, op=ALU.add), reads=[b_tmp], writes=[bdst])
                    S.op("dve", lambda e: e.tensor_tensor(out=tmp[:], in0=oh[:], in1=gts[:], op=ALU.mult), reads=[b_oh, b_gts], writes=[b_tmp])
                    S.op("dve", lambda e, j=j, g=g: e.tensor_reduce(out=gate4[:, 4 * g:4 * g + 4, j], in_=tmp[:], axis=AX.X, op=ALU.add),
                         reads=[b_tmp], writes=[b_gate4])
                S.op("dve", lambda e, g=g: e.tensor_copy(slot4s[:, 4 * g:4 * g + 4, :], f4s[:]), reads=[b_f4s], writes=[b_slot4s])
                S.op("dve", lambda e, g=g: e.tensor_copy(slot4g[:, 4 * g:4 * g + 4, :], f4g[:]), reads=[b_f4g], writes=[b_slot4g])
                for s in range(4):
                    t = 4 * g + s
                    for j in range(4):
                        S.dma("pool", lambda e, t=t, j=j: e.indirect_dma_start(
                            out=tab, out_offset=bass.IndirectOffsetOnAxis(ap=slot4s[:, t, j:j + 1], axis=0),
                            in_=tokid[:, t, :], in_offset=None, bounds_check=breg, oob_is_err=False),
                            "scat", reads=[b_slot4s, b_tokid, b_tab])

            def finish_counts():
                S.op("dve", lambda e: e.tensor_scalar(out=pos[0:1, 0, :], in0=base[0:1, :], scalar1=-1.0, scalar2=float(SEQ), op0=ALU.mult, op1=ALU.add),
                     reads=[b_base], writes=[b_pos])
                S.op("dve", lambda e: e.tensor_copy(cntv[:], pos[0:1, 0, :]), reads=[b_pos], writes=[b_cntv])

            proj(0)
            attn(0)
            proj(1)
            for g in range(1, NG):
                attn(g, inject=lambda h, g=g: [(jj, f, None) for jj, f in zip(WO_J, wo_stages(g - 1, h, bank=7))])
                if g + 1 < NG:
                    proj(g + 1)
                    if g + 1 == NG - 1:
                        dead = b_win + b_xT
                        for hh in range(2):
                            S.dma("pool", lambda e, hh=hh: e.dma_start(
                                out=wup0[:, :, hh * D:(hh + 1) * D],
                                in_=w_up[0, :, hh * D:(hh + 1) * D].rearrange("(c p) n -> p c n", p=128)),
                                "wld", writes=[b_wup[0][hh]] + dead, per=f"u0{hh}")
                        S.dma("pool", lambda e: e.dma_start(out=wdn0, in_=w_dn[0].rearrange("(c p) n -> p c n", p=128)),
                              "wld", writes=[b_wdn[0]] + dead, per="d0")
                routing(g - 1)
            wo_ln(NG - 1)
            routing(NG - 1)
            finish_counts()
            S.barrier()

        rot["list"] = list(range(8))
        with ExitStack() as p5:
            wup1 = p5.enter_context(nc.sbuf_tensor("wup1", [128, 8, 2 * D], BF16))
            wdn1 = p5.enter_context(nc.sbuf_tensor("wdn1", [128, 8, D], BF16))
            wup = [wup0, wup1[:]]
            wdn = [(wdn0, b_wdn[0]), (wdn1[:], b_wdn[1])]
            bd = [T(p5, f"bd{i}", [1, D], BF16) for i in range(2)]
            idx = [T(p5, f"idx{i}", [128, NB], I32) for i in range(2)]
            xg = [[T(p5, f"xg{i}_{b}", [128, D], BF16) for b in range(NB)] for i in range(1)]
            xgT = [p5.enter_context(nc.sbuf_tensor(f"xgT{i}", [128, 8, C], BF16)) for i in range(2)]
            b_xgT = [[Buf(f"xgT{i}_{b}") for b in range(NB)] for i in range(2)]
            actT = [p5.enter_context(nc.sbuf_tensor(f"actT{i}", [128, 8, C], BF16)) for i in range(2)]
            b_actT = [[[Buf(f"actT{i}_{m}_{gi}") for gi in range(2)] for m in range(8)] for i in range(2)]
            gt = [T(p5, f"gt{i}", [128, 512], F32) for i in range(2)]
            sg = [T(p5, f"sg{i}", [128, 512], F32) for i in range(2)]
            lt = [T(p5, f"lt{i}", [128, 512], F32) for i in range(2)]
            ysb = [T(p5, f"ysb{i}", [128, D], BF16) for i in range(2)]
            b_yall = Buf("yall")

            def load_w(e_, big=True):
                i = e_ % 2
                if big:
                    for hh in range(2):
                        S.dma("pool", lambda e, hh=hh: e.dma_start(
                            out=wup[i][:, :, hh * D:(hh + 1) * D],
                            in_=w_up[e_, :, hh * D:(hh + 1) * D].rearrange("(c p) n -> p c n", p=128)),
                            "wld", writes=[b_wup[i][hh]], per=f"u{i}{hh}")
                    S.dma("pool", lambda e: e.dma_start(out=wdn[i][0], in_=w_dn[e_].rearrange("(c p) n -> p c n", p=128)),
                          "wld", writes=[wdn[i][1]], per=f"d{i}")
                S.dma("pool", lambda e: e.dma_start(out=bd[i][0][:], in_=b_dn[e_:e_ + 1, :]), "wld", writes=[bd[i][1]], per=f"b{i}")

            def load_idx(e_):
                i = e_ % 2
                S.dma("pool", lambda e: e.dma_start(out=idx[i][0][:], in_=tab[e_ * C:(e_ + 1) * C, 0:1].rearrange("(b p) o -> p (b o)", p=128)),
                      "idxld", writes=[idx[i][1]], per=i)

            def load_x(e_):
                i = e_ % 2
                for b in range(NB):
                    S.dma("pool", lambda e, b=b: e.indirect_dma_start(
                        out=xg[0][b][0][:], out_offset=None, in_=x1b,
                        in_offset=bass.IndirectOffsetOnAxis(ap=idx[i][0][:, b:b + 1], axis=0)),
                        "xgat", reads=[idx[i][1]], writes=[xg[0][b][1]], per=b)

            units = [(0, 512, None), (512, 768, 512), (768, 1024, 768)]
            b_actTu = [[[Buf(f"actT{i}_{m}_{u}") for u in range(3)] for m in range(8)] for i in range(2)]
            cregs = {name: eng.alloc_register(f"cnt_{name}") for name, eng in S.engs.items()}
            state = {"k": 0, "y": 0}

            def tr_unit(e_, u):
                i = e_ % 2
                n0, n1, _ = units[u]
                for b in range(n0 // 128, n1 // 128):
                    bk, bb = nb()
                    bkb = bk[:].bitcast(BF16)
                    for c in range(8):
                        tp(bkb[:, c * 128:(c + 1) * 128], xg[0][b][0][:, c * 128:(c + 1) * 128], ident[:],
                           [xg[0][b][1], b_ident], [bb], c == 7)
                    if b % 2 == 0:
                        S.op("act", lambda e, b=b, bkb=bkb: e.activation(xgT[i][:, :, b * 128:(b + 1) * 128], bkb.rearrange("p (c t) -> p c t", c=8), AF.Copy),
                             reads=[bb], writes=[b_xgT[i][b]])
                    else:
                        S.op("dve", lambda e, b=b, bkb=bkb: e.tensor_copy(xgT[i][:, :, b * 128:(b + 1) * 128], bkb.rearrange("p (c t) -> p c t", c=8)),
                             reads=[bb], writes=[b_xgT[i][b]])

            def work_unit(e_, u):
                i = e_ % 2
                n0, n1, _ = units[u]
                n = n1 - n0
                rb = [b_xgT[i][b] for b in range(n0 // 128, n1 // 128)]
                for mp in range(8):
                    bkg, bbg = nb()
                    for c in range(8):
                        mm(bkg[:, 0:n], wup[i][:, c, mp * 128:(mp + 1) * 128], xgT[i][:, c, n0:n1], c == 0, c == 7,
                           [b_wup[i][0]] + rb, [bbg], c == 7)
                    bkl, bbl = nb()
                    for c in range(8):
                        mm(bkl[:, 0:n], wup[i][:, c, D + mp * 128:D + (mp + 1) * 128], xgT[i][:, c, n0:n1], c == 0, c == 7,
                           [b_wup[i][1]] + rb, [bbl], c == 7)
                    gtt, b_gtt = gt[state["k"] % 2]
                    sgt, b_sgt = sg[state["k"] % 2]
                    ltt, b_ltt = lt[state["k"] % 2]
                    state["k"] += 1
                    S.op("dve", lambda e, gtt=gtt, bkg=bkg, mp=mp: e.tensor_scalar(out=gtt[:, 0:n], in0=bkg[:, 0:n], scalar1=bupT[:, mp, e_:e_ + 1], scalar2=7.0,
                                                          op0=ALU.add, op1=ALU.min), reads=[bbg, b_bupT], writes=[b_gtt])
                    S.op("act", lambda e, ltt=ltt, bkl=bkl, mp=mp: e.activation(ltt[:, 0:n], bkl[:, 0:n], AF.Identity, bias=bupT[:, 8 + mp, e_:e_ + 1],
                                                                               scale=1.0 / 1.702),
                         reads=[bbl, b_bupT], writes=[b_ltt])
                    S.op("act", lambda e, sgt=sgt, gtt=gtt: e.activation(sgt[:, 0:n], gtt[:, 0:n], AF.Silu, scale=1.702), reads=[b_gtt], writes=[b_sgt])
                    S.op("dve", lambda e, ltt=ltt: e.tensor_scalar(out=ltt[:, 0:n], in0=ltt[:, 0:n], scalar1=7.0 / 1.702, scalar2=-7.0 / 1.702,
                                                                  op0=ALU.min, op1=ALU.max),
                         reads=[b_ltt], writes=[b_ltt])
                    S.op("dve", lambda e, ltt=ltt, sgt=sgt, mp=mp: e.scalar_tensor_tensor(out=actT[i][:, mp, n0:n1], in0=ltt[:, 0:n], scalar=1.0 / 1.702,
                                                                                         in1=sgt[:, 0:n], op0=ALU.add, op1=ALU.mult),
                         reads=[b_ltt, b_sgt], writes=[b_actTu[i][mp][u]])
                for b in range(n0 // 128, n1 // 128):
                    yi = state["y"] % 2
                    state["y"] += 1
                    yt, b_yt = ysb[yi]
                    for half in range(2):
                        bk, bb = nb()
                        mm(bk[:, :], ones[0:1, :], bd[i][0][0:1, half * 512:(half + 1) * 512], True, False, [b_ones, bd[i][1]], [bb], False)
                        for m in range(8):
                            mm(bk[:, :], actT[i][:, m, b * 128:(b + 1) * 128], wdn[i][0][:, m, half * 512:(half + 1) * 512], False, m == 7,
                               [b_actTu[i][m][u], wdn[i][1]], [bb], m == 7)
                        if half == 0:
                            S.op("act", lambda e, yt=yt, bk=bk: e.activation(yt[:, 0:512], bk[:, :], AF.Copy), reads=[bb], writes=[b_yt])
                        else:
                            S.op("dve", lambda e, yt=yt, bk=bk: e.tensor_copy(yt[:, 512:1024], bk[:, :]), reads=[bb], writes=[b_yt])
                    r0 = e_ * C + b * 128
                    S.dma("sp", lambda e, r0=r0, yt=yt: e.dma_start(out=y_all[r0:r0 + 128, :], in_=yt[:]), "yst", reads=[b_yt], writes=[b_yall], per=yi)

            with nc.allow_non_contiguous_dma(reason="small per-expert slot index table"):
                load_idx(0)
                load_x(0)
                load_w(0, big=False)
                for e_ in range(E):
                    for name, eng in S.engs.items():
                        eng.reg_load(cregs[name], cntv[0:1, e_:e_ + 1])
                    if e_ + 1 < E:
                        load_idx(e_ + 1)
                    rot["list"], rot["p"] = [0, 1, 2, 3], 0
                    tr_unit(e_, 0)
                    for u in (1, 2):
                        rot["list"], rot["p"] = [4, 5, 6, 7], 0
                        S.cond_region(cregs, SEQ - units[u][2], lambda u=u: tr_unit(e_, u))
                    rot["list"], rot["p"] = [0, 1, 2, 3], 0
                    if e_ + 1 < E:
                        load_x(e_ + 1)
                        load_w(e_ + 1)
                    work_unit(e_, 0)
                    for u in (1, 2):
                        rot["list"], rot["p"] = [4, 5, 6, 7], 0
                        S.cond_region(cregs, SEQ - units[u][2], lambda u=u: work_unit(e_, u))
                    rot["list"], rot["p"] = [0, 1, 2, 3], 0
            S.barrier()

        with ExitStack() as p6:
            yg = [[T(p6, f"yg{i}_{j}", [128, D], BF16) for j in range(4)] for i in range(2)]
            accb = [T(p6, f"acc{i}", [128, D], F32) for i in range(2)]
            xr = [T(p6, f"xr{i}", [128, D], F32) for i in range(2)]
            st3 = [T(p6, f"st3_{i}", [128, 2, 6], F32) for i in range(2)]
            mv3 = [T(p6, f"mv3_{i}", [128, 2], F32) for i in range(2)]
            rs3 = [T(p6, f"rs3_{i}", [128, 1], F32) for i in range(2)]
            ln2g, b_ln2g = T(p6, "ln2g", [128, D], F32)
            ln2b, b_ln2b = T(p6, "ln2b", [128, D], F32)
            bcast_load(ln2g[:], ln2_g, D, b_ln2g)
            bcast_load(ln2b[:], ln2_b, D, b_ln2b)
            last_tok = None

            def gathers(t):
                i = t % 2
                for j in range(4):
                    S.dma("pool", lambda e, j=j: e.indirect_dma_start(
                        out=yg[i][j][0][:], out_offset=None, in_=y_all,
                        in_offset=bass.IndirectOffsetOnAxis(ap=slot4g[:, t, j:j + 1], axis=0)),
                        "ygat", reads=[b_slot4g], writes=[yg[i][j][1]], per=f"{i}{j}")
                S.dma("sp", lambda e: e.dma_start(out=xr[i][0][:], in_=x1f[t * 128:(t + 1) * 128, :]), "xrld", writes=[xr[i][1]], per=i)

            def part_a(t):
                i = t % 2
                xrt, b_xrt = xr[i]
                act_, b_act = accb[i]
                stt, b_stt = st3[i]
                mvt, b_mvt = mv3[i]
                rst, b_rst = rs3[i]
                S.op("act", lambda e: e.activation(act_[:], yg[i][0][0][:], AF.Copy, scale=gate4[:, t, 0:1]),
                     reads=[yg[i][0][1], b_gate4], writes=[b_act])
                for j in range(1, 4):
                    S.op("dve", lambda e, j=j: e.scalar_tensor_tensor(out=act_[:], in0=yg[i][j][0][:], scalar=gate4[:, t, j:j + 1], in1=act_[:],
                                                                      op0=ALU.mult, op1=ALU.add),
                         reads=[yg[i][j][1], b_gate4, b_act], writes=[b_act])
                S.op("dve", lambda e: e.scalar_tensor_tensor(out=act_[:], in0=xrt[:], scalar=ALPHA, in1=act_[:], op0=ALU.mult, op1=ALU.add),
                     reads=[b_xrt, b_act], writes=[b_act])
                for half in range(2):
                    S.op("dve", lambda e, half=half: e.bn_stats(stt[:, half, :], act_[:, half * 512:(half + 1) * 512]), reads=[b_act], writes=[b_stt])
                S.op("dve", lambda e: e.bn_aggr(mvt[:], stt[:]), reads=[b_stt], writes=[b_mvt])
                S.op("act", lambda e: e.activation(rst[:], mvt[:, 1:2], AF.Ln, bias=EPS, scale=1.0), reads=[b_mvt], writes=[b_rst])
                S.op("act", lambda e: e.activation(rst[:], rst[:], AF.Exp, scale=-0.5), reads=[b_rst], writes=[b_rst])

            def part_b(t):
                i = t % 2
                act_, b_act = accb[i]
                mvt, b_mvt = mv3[i]
                rst, b_rst = rs3[i]
                S.op("dve", lambda e: e.scalar_tensor_tensor(out=mvt[:, 0:1], in0=mvt[:, 0:1], scalar=-1.0, in1=rst[:, 0:1], op0=ALU.mult, op1=ALU.mult),
                     reads=[b_mvt, b_rst], writes=[b_mvt])
                S.op("act", lambda e: e.activation(act_[:], act_[:], AF.Identity, bias=mvt[:, 0:1], scale=rst[:, 0:1]),
                     reads=[b_act, b_mvt, b_rst], writes=[b_act])
                S.op("dve", lambda e: e.tensor_tensor(out=act_[:], in0=act_[:], in1=ln2g[:], op=ALU.mult), reads=[b_act, b_ln2g], writes=[b_act])
                S.op("pool", lambda e: e.tensor_tensor(out=act_[:], in0=act_[:], in1=ln2b[:], op=ALU.add), reads=[b_act, b_ln2b], writes=[b_act])
                S.dma("sp", lambda e: e.dma_start(out=out[t * 128:(t + 1) * 128, :], in_=act_[:]), "ost", reads=[b_act], per=i)

            gathers(0)
            gathers(1)
            part_a(0)
            for t in range(NT):
                if t + 1 < NT:
                    part_a(t + 1)
                if t + 2 < NT:
                    gathers(t + 2)
                part_b(t)
            S.barrier()
    return nc


_CACHE = {}


def _consts():
    p = np.arange(128)
    c = {}
    c["c_ident"] = np.eye(128, dtype=np.float32)
    c["c_maskT"] = (p[:, None] <= p[None, :]).astype(np.float32)
    c["c_tri"] = (p[:, None] < p[None, :]).astype(np.float32)
    c["c_ones"] = np.ones((128, 128), np.float32)
    c["c_tril"] = (p[None, :] <= p[:, None]).astype(np.float32)
    c["c_tokid"] = np.broadcast_to((np.arange(NT)[None, :] * 128 + p[:, None]).astype(np.int32)[:, :, None], (128, NT, TABW)).copy()
    c["c_ecb"] = np.broadcast_to((np.arange(E) * C).astype(np.float32)[None, None, :], (128, 4, E)).copy()
    return c


def kernel(**inputs):
    if "nc" not in _CACHE:
        _CACHE["nc"] = build()
    nc = _CACHE["nc"]
    f = lambda a: np.ascontiguousarray(np.asarray(a, dtype=np.float32))
    shared = {
        "w_in": f(inputs["w_in"][0]),
        "lambda_q1": f(inputs["lambda_q1"]), "lambda_k1": f(inputs["lambda_k1"]),
        "lambda_q2": f(inputs["lambda_q2"]), "lambda_k2": f(inputs["lambda_k2"]),
        "subln_g": f(inputs["subln_g"]),
        "gmlp_ln_g": f(inputs["gmlp_ln_g"]), "gmlp_ln_b": f(inputs["gmlp_ln_b"]),
        "w_spatial": f(inputs["w_spatial"][0]), "b_spatial": f(inputs["b_spatial"][0]),
        "w_o": f(inputs["w_o"][0]),
        "ln1_g": f(inputs["ln1_g"]), "ln1_b": f(inputs["ln1_b"]),
        "w_router": f(inputs["w_router"][0]), "b_router": f(inputs["b_router"]),
        "w_up": f(inputs["w_up"][0]), "b_up": f(inputs["b_up"][0]),
        "w_down": f(inputs["w_down"][0]), "b_down": f(inputs["b_down"][0]),
        "ln2_g": f(inputs["ln2_g"]), "ln2_b": f(inputs["ln2_b"]),
    }
    shared.update(_consts())
    xs = np.asarray(inputs["x"], dtype=np.float32)
    in_maps = [dict(shared, x=np.ascontiguousarray(xs[b])) for b in range(N_CORES)]
    res = run_bass_kernel_spmd(nc, in_maps, core_ids=list(range(N_CORES)))
    return np.stack([np.asarray(r["out"], dtype=np.float32) for r in res.results], axis=0)
```

```python
import numpy as np
from contextlib import ExitStack
import concourse.bass as bass
import concourse.mybir as mybir
from concourse.bass_utils import run_bass_kernel_spmd

F32 = mybir.dt.float32
BF16 = mybir.dt.bfloat16
I32 = mybir.dt.int32
AF = mybir.ActivationFunctionType
ALU = mybir.AluOpType
AX = mybir.AxisListType

D = 1024
SEQ = 4096
NT = 32
NG = 8
E = 32
C = 1024
NB = C // 128
NSLOT = E * C
TABW = 16
ZROW = NSLOT
BIG = float(NSLOT + 128)
ALPHA = 2.0 ** 0.25
LAMBDA_INIT = 0.2
EPS = 1e-5
N_CORES = 8


class Buf:
    __slots__ = ("name", "w", "r")

    def __init__(self, name):
        self.name = name
        self.w = None
        self.r = []


class Sched:
    def __init__(self, nc, es):
        self.nc = nc
        self.es = es
        self.engs = {"pe": nc.tensor, "act": nc.scalar, "dve": nc.vector, "pool": nc.gpsimd, "sp": nc.sync}
        self.sems = {}
        self.cnt = {}
        self.waited = {k: {} for k in self.engs}
        self.issuer = {}
        self.rec = None
        self.ring = 0
        for k in self.engs:
            self.sems[k] = es.enter_context(nc.semaphore("c_" + k))
            self.cnt[k] = 0

    def _wait(self, eng, tok, out=None):
        if tok is None:
            return
        key, val = tok
        if key == eng and eng in ("pe", "sp"):
            return
        if self.waited[eng].get(key, 0) >= val:
            return
        self.waited[eng][key] = val
        if out is None:
            self.engs[eng].wait_ge(self.sems[key], val)
        else:
            out.append((key, val))

    def _deps(self, eng, reads, writes):
        need = {}
        toks = [b.w for b in reads] + [b.w for b in writes] + [t for b in writes for t in b.r]
        for t in toks:
            if t is not None and need.get(t[0], 0) < t[1]:
                need[t[0]] = t[1]
        out = []
        for k, v in need.items():
            self._wait(eng, (k, v), out)
        return out

    def _commit(self, tok, reads, writes):
        for b in reads:
            b.r.append(tok)
            if len(b.r) > 24:
                best = {}
                for (k, v) in b.r:
                    if best.get(k, 0) < v:
                        best[k] = v
                b.r = list(best.items())
        for b in writes:
            b.w = tok
            b.r = []

    def _emit(self, eng, waits, fn, sem, inc):
        def go():
            e = self.engs[eng]
            for (k, v) in waits:
                e.wait_ge(self.sems[k], v)
            ins = fn(e)
            if sem is not None:
                ins.then_inc(self.sems[sem], inc)
        if self.rec is not None:
            self.rec.append((eng, go))
        else:
            go()

    def op(self, eng, fn, reads=(), writes=(), sig=True):
        waits = self._deps(eng, reads, writes)
        if sig:
            self.cnt[eng] += 1
            tok = (eng, self.cnt[eng])
        else:
            tok = (eng, self.cnt[eng] + 1)
        self._emit(eng, waits, fn, eng if sig else None, 1)
        self._commit(tok, reads, writes)
        return tok

    def dma(self, eng, fn, sem, reads=(), writes=(), per=None):
        if per is not None:
            sem = f"{sem}_{per}"
        elif sem == "setup":
            if eng == "pool":
                sem = f"setup_pool{self.ring % 8}"
                self.ring += 1
            else:
                sem = f"setup_{eng}"
        if sem not in self.sems:
            self.sems[sem] = self.es.enter_context(self.nc.semaphore("d_" + sem))
            self.cnt[sem] = 0
        waits = self._deps(eng, reads, writes)
        self.issuer.setdefault(sem, set()).add(eng)
        self.cnt[sem] += 16
        tok = (sem, self.cnt[sem])
        self._emit(eng, waits, fn, sem, 16)
        self._commit(tok, reads, writes)
        return tok

    def cond_region(self, regs, bound, body):
        snap = dict(self.cnt)
        snap_waited = {k: dict(v) for k, v in self.waited.items()}
        assert self.rec is None
        self.rec = []
        body()
        rec, self.rec = self.rec, None
        delta = {k: self.cnt[k] - snap.get(k, 0) for k in self.cnt if self.cnt[k] != snap.get(k, 0)}
        assert all(k in self.engs or len(self.issuer[k]) == 1 for k in delta)
        for name, eng in self.engs.items():
            thunks = [go for (en, go) in rec if en == name]
            mine = [k for k in delta if k == name or self.issuer.get(k) == {name}]
            if not thunks and not mine:
                continue
            with eng.If_lt(regs[name], bound):
                for go in thunks:
                    go()
            with eng.Else():
                for k in mine:
                    if snap.get(k, 0) > 0:
                        eng.wait_ge(self.sems[k], snap.get(k, 0))
                    eng.sem_inc(self.sems[k], delta[k])
        self.waited = snap_waited

    def barrier(self):
        for eng in self.engs:
            for key in list(self.sems):
                if self.cnt[key] > 0:
                    self._wait(eng, (key, self.cnt[key]))


def build(debug=False):
    nc = bass.Bass("TRN2", target_bir_lowering=False)

    def din(name, shape, dtype=F32):
        return nc.dram_tensor(name, shape, dtype, kind="ExternalInput").ap()

    x = din("x", [SEQ, D])
    w_in = din("w_in", [D, 2560])
    lq1 = din("lambda_q1", [1, 64]); lk1 = din("lambda_k1", [1, 64])
    lq2 = din("lambda_q2", [1, 64]); lk2 = din("lambda_k2", [1, 64])
    subln_g = din("subln_g", [1, 128])
    gln_g = din("gmlp_ln_g", [1, 512]); gln_b = din("gmlp_ln_b", [1, 512])
    w_sp = din("w_spatial", [8, 128, 128]); b_sp = din("b_spatial", [8, 128])
    w_o = din("w_o", [D, D])
    ln1_g = din("ln1_g", [1, D]); ln1_b = din("ln1_b", [1, D])
    w_r = din("w_router", [D, E]); b_r = din("b_router", [1, E])
    w_up = din("w_up", [E, D, 2 * D]); b_up = din("b_up", [E, 2 * D])
    w_dn = din("w_down", [E, D, D]); b_dn = din("b_down", [E, D])
    ln2_g = din("ln2_g", [1, D]); ln2_b = din("ln2_b", [1, D])
    c_ident = din("c_ident", [128, 128])
    c_maskT = din("c_maskT", [128, 128])
    c_tri = din("c_tri", [128, 128])
    c_ones = din("c_ones", [128, 128])
    c_tril = din("c_tril", [128, 128])
    c_tokid = din("c_tokid", [128, NT, TABW], I32)
    c_ecb = din("c_ecb", [128, 4, E])

    out = nc.dram_tensor("out", [SEQ, D], F32, kind="ExternalOutput").ap()
    skind = "ExternalOutput" if debug else "Internal"
    x1f = nc.dram_tensor("x1f", [SEQ, D], F32, kind=skind).ap()
    x1b = nc.dram_tensor("x1b", [SEQ, D], BF16, kind=skind).ap()
    tab = nc.dram_tensor("tab", [NSLOT, TABW], I32, kind=skind).ap()
    y_all = nc.dram_tensor("y_all", [NSLOT + 1, D], BF16, kind=skind).ap()
    if debug:
        dbgA = nc.dram_tensor("dbgA", [NG, 128, 4, 512], BF16, kind="ExternalOutput").ap()
        dbgG = nc.dram_tensor("dbgG", [NG, 128, 4, 512], BF16, kind="ExternalOutput").ap()

    with ExitStack() as es:
        S = Sched(nc, es)
        banks = [es.enter_context(nc.psum_tensor(f"bk{i}", [128, 512], F32)) for i in range(8)]
        bbuf = [Buf(f"bk{i}") for i in range(8)]
        rot = {"list": [0, 1, 2, 3], "p": 0}

        def nb():
            i = rot["list"][rot["p"] % len(rot["list"])]
            rot["p"] += 1
            return banks[i], bbuf[i]

        def T(stack, name, shape, dtype):
            return stack.enter_context(nc.sbuf_tensor(name, shape, dtype)), Buf(name)

        def mm(out_ap, lhsT, rhs, start, stop, reads, writes, sig, sgc=False):
            return S.op("pe", lambda e: e.matmul(out_ap, lhsT, rhs, start=start, stop=stop, skip_group_check=sgc),
                        reads=reads, writes=writes, sig=sig)

        def tp(out_ap, in_ap, ident_ap, reads, writes, sig):
            return S.op("pe", lambda e: e.transpose(out_ap, in_ap, ident_ap), reads=reads, writes=writes, sig=sig)

        def bcast_load(dst, src, n, wbuf, eng="sp"):
            S.dma(eng, lambda e: e.dma_start(out=dst, in_=src.to_broadcast([128, n])), "setup", writes=[wbuf])

        ident, b_ident = T(es, "ident", [128, 128], BF16)
        ones, b_ones = T(es, "ones", [128, 128], BF16)
        onesf, b_onesf = T(es, "onesf", [33, 128], F32)
        tokid, b_tokid = T(es, "tokid", [128, NT, TABW], I32)
        zer, b_zer = T(es, "zer", [1, 512], BF16)
        big0 = es.enter_context(nc.sbuf_tensor("big0", [128, 24576], BF16))
        win = big0[:, 0:20480].rearrange("p (c n) -> p c n", c=8)
        xT = big0[:, 20480:24576].rearrange("p (c n) -> p c n", c=8)
        wup0 = big0[:, 0:16384].rearrange("p (c n) -> p c n", c=8)
        wdn0 = big0[:, 16384:24576].rearrange("p (c n) -> p c n", c=8)
        b_wup = [[Buf(f"wup{i}_{hh}") for hh in range(2)] for i in range(2)]
        b_wdn = [Buf("wdn0"), Buf("wdn1")]
        cntv, b_cntv = T(es, "cntv", [1, E], I32)
        S.op("pool", lambda e: e.memset(zer[:], 0.0), writes=[b_zer])
        slot4s, b_slot4s = T(es, "slot4s", [128, NT, 4], I32)
        slot4g, b_slot4g = T(es, "slot4g", [128, NT, 4], I32)
        gate4, b_gate4 = T(es, "gate4", [128, NT, 4], F32)
        bupT, b_bupT = T(es, "bupT", [128, 16, E], F32)
        S.dma("pool", lambda e: e.dma_start(out=ident[:], in_=c_ident), "setup", writes=[b_ident])
        S.dma("pool", lambda e: e.dma_start(out=ones[:], in_=c_ones), "setup", writes=[b_ones])
        S.dma("sp", lambda e: e.dma_start(out=onesf[:], in_=c_ones[0:33, :]), "setup", writes=[b_onesf])
        S.dma("sp", lambda e: e.dma_start(out=tokid[:], in_=c_tokid), "setup", writes=[b_tokid])

        with ExitStack() as p1:
            b_win = [Buf(f"win{j}") for j in range(5)]
            wo, b_wo = T(p1, "wo", [128, 8, D], BF16)
            wr, b_wr = T(p1, "wr", [128, 8, E], BF16)
            br, b_br = T(p1, "br", [128, E], F32)
            maskT, b_maskT = T(p1, "maskT", [128, 128], BF16)
            tri, b_tri = T(p1, "tri", [128, 128], BF16)
            ecb, b_ecb = T(p1, "ecb", [128, 4, E], F32)
            glng, b_glng = T(p1, "glng", [128, 512], F32)
            glnb, b_glnb = T(p1, "glnb", [128, 512], F32)
            ln1g, b_ln1g = T(p1, "ln1g", [128, D], F32)
            ln1b, b_ln1b = T(p1, "ln1b", [128, D], F32)
            g08c, b_g08c = T(p1, "g08c", [128, 1], F32)
            neglam, b_neglam = T(p1, "neglam", [128, 1], F32)
            WT, b_WT = T(p1, "WT", [128, 8, 128], BF16)
            bsT, b_bsT = T(p1, "bsT", [128, 8], F32)
            KT = p1.enter_context(nc.sbuf_tensor("KT", [128, 4, SEQ], BF16))
            b_KT = [[Buf(f"KT{c}_{g}") for g in range(NG)] for c in range(4)]
            Vsb = p1.enter_context(nc.sbuf_tensor("Vsb", [128, NT, 4, 129], BF16))
            b_V = [Buf(f"V{t}") for t in range(NT)]
            QT = p1.enter_context(nc.sbuf_tensor("QT", [128, 4, 512], BF16))
            b_QT = [Buf(f"QT{c}") for c in range(4)]
            b_xT = [Buf(f"xT{s}") for s in range(4)]
            GTs = [p1.enter_context(nc.sbuf_tensor(f"GT{i}", [128, 4, 512], BF16)) for i in range(2)]
            b_GTs = [[Buf(f"GT{i}_{s}") for s in range(4)] for i in range(2)]
            ATs = [p1.enter_context(nc.sbuf_tensor(f"AT{i}", [128, 4, 512], BF16)) for i in range(2)]
            b_ATs = [[Buf(f"AT{i}_{h}") for h in range(4)] for i in range(2)]
            p0 = ExitStack()
            identf, b_identf = T(p0, "identf", [128, 128], F32)
            S.dma("sp", lambda e: e.dma_start(out=identf[:], in_=c_ident), "setup", writes=[b_identf])
            lam_t, b_lam_t = T(p0, "lam_t", [128, 4, 64], F32)
            lam_s, b_lam_s = T(p0, "lam_s", [128, 2], F32)
            wsp, b_wsp = T(p0, "wsp", [128, 8, 128], F32)
            wspb, b_wspb = T(p0, "wspb", [128, 8, 128], BF16)
            trilt, b_trilt = T(p0, "trilt", [128, 128], F32)
            bsp8, b_bsp8 = T(p0, "bsp8", [8, 128], F32)
            bup_sb, b_bup_sb = T(p0, "bup_sb", [E, 2 * D], F32)
            ztab, b_ztab = T(p0, "ztab", [128, 64 * TABW], I32)
            zrow, b_zrow = T(p0, "zrow", [1, D], BF16)
            for j in range(2):
                for tt in range(2):
                    for hh in range(4):
                        S.dma("pool", lambda e, j=j, tt=tt, hh=hh: e.dma_start(
                            out=win[:, :, j * 512 + hh * 128 + tt * 64:j * 512 + hh * 128 + (tt + 1) * 64],
                            in_=w_in[:, j * 512 + tt * 256 + hh * 64:j * 512 + tt * 256 + (hh + 1) * 64].rearrange("(c p) d -> p c d", p=128)),
                            "setup", writes=[b_win[j]])
            for j in range(2, 5):
                S.dma("pool", lambda e, j=j: e.dma_start(
                    out=win[:, :, j * 512:(j + 1) * 512],
                    in_=w_in[:, j * 512:(j + 1) * 512].rearrange("(c p) n -> p c n", p=128)),
                    "setup", writes=[b_win[j]])
            S.dma("pool", lambda e: e.dma_start(out=maskT[:], in_=c_maskT), "setup", writes=[b_maskT])
            S.dma("pool", lambda e: e.dma_start(out=tri[:], in_=c_tri), "setup", writes=[b_tri])
            S.dma("sp", lambda e: e.dma_start(out=ecb[:], in_=c_ecb), "setup", writes=[b_ecb])
            S.dma("sp", lambda e: e.dma_start(out=trilt[:], in_=c_tril), "setup", writes=[b_trilt])
            S.dma("sp", lambda e: e.dma_start(out=wsp[:], in_=w_sp.rearrange("g t s -> t g s")), "setup", writes=[b_wsp])
            S.dma("sp", lambda e: e.dma_start(out=bsp8[:], in_=b_sp), "setup", writes=[b_bsp8])
            S.dma("sp", lambda e: e.dma_start(out=bup_sb[:], in_=b_up), "setup", writes=[b_bup_sb])
            bcast_load(glng[:], gln_g, 512, b_glng)
            bcast_load(glnb[:], gln_b, 512, b_glnb)
            bcast_load(ln1g[:], ln1_g, D, b_ln1g)
            bcast_load(ln1b[:], ln1_b, D, b_ln1b)
            with nc.allow_non_contiguous_dma(reason="128-element per-partition gain column"):
                S.dma("sp", lambda e: e.dma_start(out=g08c[:], in_=subln_g.rearrange("o n -> n o")), "setup", writes=[b_g08c])
            bcast_load(br[:], b_r, E, b_br)
            for i, src in enumerate([lq1, lk1, lq2, lk2]):
                bcast_load(lam_t[:, i, :], src, 64, b_lam_t)
            S.dma("pool", lambda e: e.dma_start(out=wo[:], in_=w_o.rearrange("(c p) n -> p c n", p=128)),
                  "setup", writes=[b_wo])
            S.dma("pool", lambda e: e.dma_start(out=wr[:], in_=w_r.rearrange("(c p) n -> p c n", p=128)),
                  "setup", writes=[b_wr])

            S.barrier()
            S.op("pool", lambda e: e.memset(ztab[:], 0), writes=[b_ztab])
            S.op("pool", lambda e: e.memset(zrow[:], 0.0), writes=[b_zrow])
            b_tab = Buf("tab")
            for q in range(NSLOT // (128 * 64)):
                S.dma("sp", lambda e, q=q: e.dma_start(out=tab[q * 8192:(q + 1) * 8192, :].rearrange("(p c) w -> p (c w)", p=128), in_=ztab[:]),
                      "setup", reads=[b_ztab], writes=[b_tab], per="ztab")
            S.dma("sp", lambda e: e.dma_start(out=y_all[ZROW:ZROW + 1, :], in_=zrow[:]), "setup", reads=[b_zrow], per="zrow")

            S.op("dve", lambda e: e.tensor_tensor(out=lam_t[:, 0, :], in0=lam_t[:, 0, :], in1=lam_t[:, 1, :], op=ALU.mult),
                 reads=[b_lam_t], writes=[b_lam_t])
            S.op("dve", lambda e: e.tensor_tensor(out=lam_t[:, 2, :], in0=lam_t[:, 2, :], in1=lam_t[:, 3, :], op=ALU.mult),
                 reads=[b_lam_t], writes=[b_lam_t])
            S.op("dve", lambda e: e.tensor_reduce(out=lam_s[:, 0:1], in_=lam_t[:, 0, :], axis=AX.X, op=ALU.add),
                 reads=[b_lam_t], writes=[b_lam_s])
            S.op("dve", lambda e: e.tensor_reduce(out=lam_s[:, 1:2], in_=lam_t[:, 2, :], axis=AX.X, op=ALU.add),
                 reads=[b_lam_t], writes=[b_lam_s])
            S.op("act", lambda e: e.activation(lam_s[:], lam_s[:], AF.Exp), reads=[b_lam_s], writes=[b_lam_s])
            S.op("dve", lambda e: e.scalar_tensor_tensor(out=neglam[:], in0=lam_s[:, 1:2], scalar=-LAMBDA_INIT,
                                                         in1=lam_s[:, 0:1], op0=ALU.add, op1=ALU.subtract),
                 reads=[b_lam_s], writes=[b_neglam])
            S.op("dve", lambda e: e.tensor_scalar(out=g08c[:], in0=g08c[:], scalar1=1.0 - LAMBDA_INIT, scalar2=None, op0=ALU.mult),
                 reads=[b_g08c], writes=[b_g08c])
            for gq in range(8):
                S.op("dve", lambda e, gq=gq: e.tensor_tensor(out=wspb[:, gq, :], in0=wsp[:, gq, :], in1=trilt[:], op=ALU.mult),
                     reads=[b_wsp, b_trilt], writes=[b_wspb])
            bk, bb = nb()
            bkb = bk[:].bitcast(BF16)
            for gq in range(8):
                tp(bkb[:, gq * 128:(gq + 1) * 128], wspb[:, gq, :], ident[:], [b_wspb, b_ident], [bb], gq == 7)
            S.op("dve", lambda e: e.tensor_copy(WT[:], bkb.rearrange("p (g t) -> p g t", g=8)), reads=[bb], writes=[b_WT])
            bk, bb = nb()
            tp(bk[:, 0:8], bsp8[:, :], identf[0:8, 0:8], [b_bsp8, b_identf], [bb], True)
            S.op("dve", lambda e: e.tensor_copy(bsT[:], bk[:, 0:8]), reads=[bb], writes=[b_bsT])
            bk, bb = nb()
            for m in range(16):
                tp(bk[:, m * E:(m + 1) * E], bup_sb[:, m * 128:(m + 1) * 128], identf[0:E, 0:E], [b_bup_sb, b_identf], [bb], m == 15)
            S.op("dve", lambda e: e.tensor_copy(bupT[:], bk[:].rearrange("p (m e) -> p m e", m=16)), reads=[bb], writes=[b_bupT])
            S.op("dve", lambda e: e.tensor_scalar(out=bupT[:, 8:16, :], in0=bupT[:, 8:16, :], scalar1=1.0 / 1.702, scalar2=None, op0=ALU.mult),
                 reads=[b_bupT], writes=[b_bupT])

            S.barrier()
            p0.close()
            xb = [T(p1, f"xb{i}", [128, D], BF16) for i in range(1)]
            u_sb = [T(p1, f"u{i}", [128, 512], BF16) for i in range(4)]
            gvy = p1.enter_context(nc.sbuf_tensor("gvy", [128, 4, 512], F32))
            b_gvy = [Buf(f"gvy{i}") for i in range(4)]
            gv_sb = [(gvy[:, i, :], b_gvy[i]) for i in range(4)]
            yb = [(gvy[:, 2 * i:2 * i + 2, :].rearrange("p a b -> p (a b)"), [b_gvy[2 * i], b_gvy[2 * i + 1]]) for i in range(2)]
            st1 = [T(p1, f"st1_{i}", [128, 6], F32) for i in range(4)]
            mvg, b_mvg = T(p1, "mvg", [128, 4, 2], F32)
            rstdg, b_rstdg = T(p1, "rstdg", [128, 4], F32)
            vbf, b_vbf = T(p1, "vbf", [128, 512], BF16)
            gated, b_gated = T(p1, "gated", [128, 512], BF16)
            pT = [[T(p1, f"pT{t}{k}", [128, 512], BF16) for k in range(2)] for t in range(2)]
            OTs = [p1.enter_context(nc.sbuf_tensor(f"OTs{i}", [128, 512], F32)) for i in range(2)]
            b_OTs = [Buf("OTs0"), Buf("OTs1")]
            accs_f, b_accs = OTs[0], b_OTs[0]
            Lsb = p1.enter_context(nc.sbuf_tensor("Lsb", [33, 512], F32))
            b_Lsb = [Buf("Lsb0"), Buf("Lsb1")]
            b_L = [Buf("L0"), Buf("L1")]
            sqb, b_sqb = T(p1, "sqb", [128, 512], BF16)
            st2 = [T(p1, f"st2_{i}", [128, 2, 6], F32) for i in range(4)]
            mv1, b_mv1 = T(p1, "mv1", [128, 4, 2], F32)
            rstd1, b_rstd1 = T(p1, "rstd1", [128, 4], F32)
            x1bf, b_x1bf = T(p1, "x1bf", [128, D], BF16)
            x1T, b_x1T = T(p1, "x1T", [128, 8, 128], BF16)
            lg, b_lg = T(p1, "lg", [128, 4, E], F32)
            m8, b_m8 = T(p1, "m8", [128, 4, 8], F32)
            msk, b_msk = T(p1, "msk", [128, 4, E], F32)
            mskb, b_mskb = T(p1, "mskb", [128, 4, E], BF16)
            ex, b_ex = T(p1, "ex", [128, 4, E], F32)
            gts, b_gts = T(p1, "gts", [128, 4, E], F32)
            pos, b_pos = T(p1, "pos", [128, 4, E], F32)
            okf, b_okf = T(p1, "okf", [128, 4, E], F32)
            sls, b_sls = T(p1, "sls", [128, 4, E], F32)
            slg, b_slg = T(p1, "slg", [128, 4, E], F32)
            oh, b_oh = msk, b_msk
            tmp, b_tmp = ex, b_ex
            base, b_base = T(p1, "base", [128, E], F32)
            sm4, b_sm4 = T(p1, "sm4", [128, 4], F32)
            f4s, b_f4s = T(p1, "f4s", [128, 4, 4], F32)
            f4g, b_f4g = T(p1, "f4g", [128, 4, 4], F32)

            acc_bufs = [Buf(f"acc{a}") for a in range(8)]
            breg = nc.gpsimd.to_reg(NSLOT - 1)
            S.op("pool", lambda e: e.memset(base[:], 0.0), writes=[b_base])

            def acc_ap(a, lo, hi):
                bank = banks[4 + a // 3]
                off = (a % 3) * 129
                return bank[:, off + lo:off + hi]

            pcount = [0, 0]
            b_x1f_d = Buf("x1f_d")
            b_x1b_d = Buf("x1b_d")

            def proj(g):
                for s in range(4):
                    t = 4 * g + s
                    xbt, b_xbt = xb[0]
                    S.dma("pool", lambda e, t=t, xbt=xbt: e.dma_start(out=xbt[:], in_=x[t * 128:(t + 1) * 128, :]),
                          "xld", writes=[b_xbt])
                    bk, bb = nb()
                    bkb = bk[:].bitcast(BF16)
                    for c in range(8):
                        tp(bkb[:, c * 128:(c + 1) * 128], xbt[:, c * 128:(c + 1) * 128], ident[:], [b_xbt, b_ident], [bb], c == 7)
                    S.op("dve", lambda e, s=s, bkb=bkb: e.tensor_copy(xT[:, :, s * 128:(s + 1) * 128],
                                                                     bkb.rearrange("p (c t) -> p c t", c=8)),
                         reads=[bb], writes=[b_xT[s]])
                for s in range(4):
                    t = 4 * g + s
                    bk, bb = nb()
                    for c in range(8):
                        mm(bk[:, :], xT[:, c, s * 128:(s + 1) * 128], win[:, c, 1024:1536], c == 0, c == 7,
                           [b_win[2], b_xT[s]], [bb], c == 7)
                    S.op("dve", lambda e, t=t, bk=bk: e.tensor_copy(Vsb[:, t, :, 0:128], bk[:].rearrange("p (h d) -> p h d", h=4)),
                         reads=[bb], writes=[b_V[t]])
                    bk, bb = nb()
                    for c in range(8):
                        mm(bk[:, :], xT[:, c, s * 128:(s + 1) * 128], win[:, c, 1536:2048], c == 0, c == 7,
                           [b_win[3], b_xT[s]], [bb], c == 7)
                    ut, b_ut = u_sb[s]
                    S.op("act", lambda e, ut=ut, bk=bk: e.activation(ut[:], bk[:, :], AF.Gelu), reads=[bb], writes=[b_ut])
                    bk, bb = nb()
                    for c in range(8):
                        mm(bk[:, :], xT[:, c, s * 128:(s + 1) * 128], win[:, c, 2048:2560], c == 0, c == 7,
                           [b_win[4], b_xT[s]], [bb], c == 7)
                    gvt, b_gvt = gv_sb[s]
                    S.op("act", lambda e, gvt=gvt, bk=bk: e.activation(gvt, bk[:, :], AF.Gelu), reads=[bb], writes=[b_gvt])
                    stt, b_stt = st1[s]
                    S.op("dve", lambda e, stt=stt, gvt=gvt: e.bn_stats(stt[:], gvt), reads=[b_gvt], writes=[b_stt])
                    S.op("dve", lambda e, s=s, stt=stt: e.bn_aggr(mvg[:, s, :], stt[:]), reads=[b_stt], writes=[b_mvg])
                S.op("act", lambda e: e.activation(rstdg[:], mvg[:, :, 1], AF.Ln, bias=EPS, scale=1.0), reads=[b_mvg], writes=[b_rstdg])
                S.op("act", lambda e: e.activation(rstdg[:], rstdg[:], AF.Exp, scale=-0.5), reads=[b_rstdg], writes=[b_rstdg])

                def fm(m):
                    bk, bb = nb()
                    for c in range(8):
                        mm(bk[:, :], win[:, c, m * 128:(m + 1) * 128], xT[:, c, :], c == 0, c == 7,
                           [b_win[m // 4]] + b_xT, [bb], c == 7)
                    if m < 4:
                        dst, wb, sc = QT[:, m, :], b_QT[m], 0.125
                    else:
                        dst, wb, sc = KT[:, m - 4, g * 512:(g + 1) * 512], b_KT[m - 4][g], 1.0
                    if m % 2 == 0:
                        S.op("act", lambda e: e.activation(dst, bk[:, :], AF.Copy, scale=sc), reads=[bb], writes=[wb])
                    else:
                        S.op("dve", lambda e: e.tensor_scalar(out=dst, in0=bk[:, :], scalar1=sc, scalar2=None, op0=ALU.mult), reads=[bb], writes=[wb])

                def g1(s):
                    gvt, b_gvt = gv_sb[s]
                    S.op("dve", lambda e: e.tensor_scalar(out=gvt, in0=gvt, scalar1=mvg[:, s, 0:1], scalar2=rstdg[:, s:s + 1],
                                                          op0=ALU.subtract, op1=ALU.mult), reads=[b_gvt, b_mvg, b_rstdg], writes=[b_gvt])
                    S.op("pool", lambda e: e.tensor_tensor(out=gvt, in0=gvt, in1=glng[:], op=ALU.mult), reads=[b_gvt, b_glng], writes=[b_gvt])
                    S.op("pool", lambda e: e.tensor_tensor(out=vbf[:], in0=gvt, in1=glnb[:], op=ALU.add), reads=[b_gvt, b_glnb], writes=[b_vbf])

                def g2(s):
                    ut, b_ut = u_sb[s]
                    bk, bb = nb()
                    for gq in range(8):
                        mm(bk[:, gq * 64:(gq + 1) * 64], WT[:, gq, :], vbf[:, gq * 64:(gq + 1) * 64], True, True,
                           [b_WT, b_vbf], [bb], gq == 7)
                    S.op("dve", lambda e: e.tensor_tensor(out=accs_f[:].rearrange("p (g d) -> p g d", g=8),
                                                          in0=bk[:].rearrange("p (g d) -> p g d", g=8),
                                                          in1=bsT[:].unsqueeze(2).to_broadcast([128, 8, 64]), op=ALU.add),
                         reads=[bb, b_bsT], writes=[b_accs])
                    S.op("dve", lambda e: e.tensor_tensor(out=gated[:], in0=accs_f[:], in1=ut[:], op=ALU.mult),
                         reads=[b_accs, b_ut], writes=[b_gated])

                def g3(s):
                    bk, bb = nb()
                    bkb = bk[:].bitcast(BF16)
                    for c in range(4):
                        tp(bkb[:, c * 128:(c + 1) * 128], gated[:, c * 128:(c + 1) * 128], ident[:], [b_gated, b_ident], [bb], c == 3)
                    S.op("dve", lambda e: e.tensor_copy(GTs[g % 2][:, :, s * 128:(s + 1) * 128],
                                                        bkb[:, 0:512].rearrange("p (c t) -> p c t", c=4)),
                         reads=[bb], writes=[b_GTs[g % 2][s]])

                for step in (g1, 0), (fm, 0), (fm, 1), (g2, 0), (g1, 1), (fm, 2), (fm, 3), (g3, 0), (g2, 1), (g1, 2), \
                        (fm, 4), (fm, 5), (g3, 1), (g2, 2), (g1, 3), (fm, 6), (fm, 7), (g3, 2), (g2, 3), (g3, 3):
                    step[0](step[1])

            def attn(g, inject=None):
                nj = 4 * g + 4
                stages = []

                def fin_evac(h):
                    S.op("dve", lambda e: e.tensor_copy(OTs[0][:], banks[4][:, :]), reads=[bbuf[4]], writes=[b_OTs[0]])
                    S.op("dve", lambda e: e.tensor_copy(OTs[1][:], banks[5][:, :]), reads=[bbuf[5]], writes=[b_OTs[1]])
                    for r0, k in ((0, 0), (32, 1)):
                        S.op("act", lambda e, r0=r0: e.activation(Lsb[r0:r0 + 1, :], banks[6][r0:r0 + 1, :], AF.Ln), reads=[b_L[k]], writes=[b_Lsb[k]])
                        S.op("act", lambda e, r0=r0: e.activation(Lsb[r0:r0 + 1, :], Lsb[r0:r0 + 1, :], AF.Exp, scale=-1.0), reads=[b_Lsb[k]], writes=[b_Lsb[k]])

                def fin_a1(h):
                    mm(banks[7][:, :], onesf[0:1, :], Lsb[0:1, :], True, True, [b_onesf, b_Lsb[0]], [bbuf[7]], True)
                    S.op("dve", lambda e: e.tensor_tensor(out=OTs[0][:], in0=OTs[0][:], in1=banks[7][:, :], op=ALU.mult),
                         reads=[b_OTs[0], bbuf[7]], writes=[b_OTs[0]])

                def fin_a2(h):
                    mm(banks[7][:, :], onesf[32:33, :], Lsb[32:33, :], True, True, [b_onesf, b_Lsb[1]], [bbuf[7]], True)
                    S.op("dve", lambda e: e.scalar_tensor_tensor(out=OTs[1][:], in0=OTs[1][:], scalar=neglam[:, 0:1], in1=banks[7][:, :],
                                                                 op0=ALU.mult, op1=ALU.mult),
                         reads=[b_OTs[1], bbuf[7], b_neglam], writes=[b_OTs[1]])
                    S.op("dve", lambda e: e.tensor_tensor(out=OTs[0][:], in0=OTs[0][:], in1=OTs[1][:], op=ALU.add),
                         reads=[b_OTs[0], b_OTs[1]], writes=[b_OTs[0]])
                    S.op("pool", lambda e: e.tensor_tensor(out=sqb[:], in0=OTs[0][:], in1=OTs[0][:], op=ALU.mult), reads=[b_OTs[0]], writes=[b_sqb])

                def fin_b(h):
                    mm(banks[7][:, :], ones[:, :], sqb[:], True, True, [b_ones, b_sqb], [bbuf[7]], True)
                    S.op("act", lambda e: e.activation(OTs[1][:], banks[7][:, :], AF.Ln, bias=EPS, scale=1.0 / 128.0), reads=[bbuf[7]], writes=[b_OTs[1]])
                    S.op("act", lambda e: e.activation(OTs[1][:], OTs[1][:], AF.Exp, scale=-0.5), reads=[b_OTs[1]], writes=[b_OTs[1]])
                    S.op("dve", lambda e: e.scalar_tensor_tensor(out=ATs[g % 2][:, h, :], in0=OTs[0][:], scalar=g08c[:, 0:1], in1=OTs[1][:],
                                                                 op0=ALU.mult, op1=ALU.mult),
                         reads=[b_OTs[0], b_OTs[1], b_g08c], writes=[b_ATs[g % 2][h]])

                for h in range(4):
                    if inject is not None:
                        stages.extend(inject(h))

                    def issue_S(j):
                        dj = j - 4 * g
                        i0 = max(dj, 0)
                        c0 = i0 * 128
                        res = []
                        for typ in range(2):
                            bi_s = 2 * (j % 2) + typ
                            bk, bb = banks[bi_s], bbuf[bi_s]
                            r0 = 64 * typ
                            mm(bk[:, c0:512], KT[r0:r0 + 64, h, j * 128:(j + 1) * 128], QT[r0:r0 + 64, h, c0:512], True, True,
                               [b_KT[h][j // 4], b_QT[h]], [bb], typ == 1)
                            res.append((bk, bb))
                        return dj, i0, c0, res

                    cur = issue_S(0)
                    for j in range(nj):
                        nxt = issue_S(j + 1) if j + 1 < nj else None
                        dj, i0, c0, res = cur
                        ps = []
                        for typ in range(2):
                            bk, bb = res[typ]
                            p, bp = pT[typ][pcount[typ] % 2]
                            pcount[typ] += 1
                            S.op("act", lambda e: e.activation(p[:, c0:512], bk[:, c0:512], AF.Exp), reads=[bb], writes=[bp])
                            if dj >= 0:
                                S.op("dve", lambda e: e.tensor_tensor(out=p[:, c0:c0 + 128], in0=p[:, c0:c0 + 128], in1=maskT[:], op=ALU.mult),
                                     reads=[bp, b_maskT], writes=[bp])
                            ps.append((p, bp))
                        allp = [ps[0][1], ps[1][1]]
                        for typ in range(2):
                            p, bp = ps[typ]
                            mm(banks[4 + typ][:, c0:512], Vsb[:, j, h, 0:128], p[:, c0:512], j == 0, j == nj - 1,
                               allp + [b_V[j]], [bbuf[4 + typ]], False)
                        for typ in range(2):
                            p, bp = ps[typ]
                            S.op("pe", lambda e: e.matmul(banks[6][32 * typ:32 * typ + 1, c0:512], ones[:, 0:1], p[:, c0:512],
                                                          start=(j == 0), stop=(j == nj - 1), tile_position=(0, 32 * typ)),
                                 reads=allp + [b_ones], writes=[b_L[typ]], sig=(typ == 1))
                        cur = nxt
                        for (jj, fn, hh) in list(stages):
                            if j == min(jj, nj - 1):
                                fn(hh)
                                stages.remove((jj, fn, hh))
                    fin_evac(h)
                    stages.extend([(1, fin_a1, h), (2, fin_a2, h), (4, fin_b, h)])
                for (jj, fn, hh) in stages:
                    fn(hh)

                if debug:
                    S.dma("sp", lambda e, g=g: e.dma_start(out=dbgA[g], in_=ATs[g % 2][:]), "dbg", reads=b_ATs[g % 2], per=f"A{g % 2}")
                    S.dma("sp", lambda e, g=g: e.dma_start(out=dbgG[g], in_=GTs[g % 2][:]), "dbg", reads=b_GTs[g % 2], per=f"G{g % 2}")
            def wo_stages(g, s, bank=None):
                t = 4 * g + s
                par = g % 2
                yt, byt = yb[s % 2]
                stt, b_stt = st2[s]

                def getbank():
                    return (banks[bank], bbuf[bank]) if bank is not None else nb()

                def half_stage(half):
                    def f(_):
                        if half == 0:
                            S.dma("sp", lambda e: e.dma_start(out=yt, in_=x[t * 128:(t + 1) * 128, :]), "xfld", writes=byt, per=s % 2)
                        bk, bb = getbank()
                        for c in range(8):
                            if c < 4:
                                lhs, rb = ATs[par][:, c, s * 128:(s + 1) * 128], b_ATs[par][c]
                            else:
                                lhs, rb = GTs[par][:, c - 4, s * 128:(s + 1) * 128], b_GTs[par][s]
                            mm(bk[:, :], lhs, wo[:, c, half * 512:(half + 1) * 512], c == 0, c == 7, [rb, b_wo], [bb], c == 7)
                        S.op("dve", lambda e: e.scalar_tensor_tensor(
                            out=yt[:, half * 512:(half + 1) * 512], in0=yt[:, half * 512:(half + 1) * 512], scalar=ALPHA, in1=bk[:, :],
                            op0=ALU.mult, op1=ALU.add), reads=byt + [bb], writes=[byt[half]])
                        S.op("dve", lambda e: e.bn_stats(stt[:, half, :], yt[:, half * 512:(half + 1) * 512]), reads=[byt[half]], writes=[b_stt])
                        if half == 1:
                            S.op("dve", lambda e: e.bn_aggr(mv1[:, s, :], stt[:]), reads=[b_stt], writes=[b_mv1])
                    return f

                def rstd_stage(_):
                    S.op("act", lambda e: e.activation(rstd1[:, s:s + 1], mv1[:, s, 1:2], AF.Ln, bias=EPS, scale=1.0), reads=[b_mv1], writes=[b_rstd1])
                    S.op("act", lambda e: e.activation(rstd1[:, s:s + 1], rstd1[:, s:s + 1], AF.Exp, scale=-0.5), reads=[b_rstd1], writes=[b_rstd1])

                def norm_stage(_):
                    S.op("dve", lambda e: e.tensor_scalar(out=yt, in0=yt, scalar1=mv1[:, s, 0:1], scalar2=rstd1[:, s:s + 1],
                                                          op0=ALU.subtract, op1=ALU.mult), reads=byt + [b_mv1, b_rstd1], writes=byt)
                    S.op("dve", lambda e: e.tensor_tensor(out=yt, in0=yt, in1=ln1g[:], op=ALU.mult), reads=byt + [b_ln1g], writes=byt)
                    S.op("dve", lambda e: e.tensor_tensor(out=yt, in0=yt, in1=ln1b[:], op=ALU.add), reads=byt + [b_ln1b], writes=byt)
                    S.op("dve", lambda e: e.tensor_copy(x1bf[:], yt), reads=byt, writes=[b_x1bf])
                    S.dma("sp", lambda e: e.dma_start(out=x1f[t * 128:(t + 1) * 128, :], in_=yt), "x1st", reads=byt, writes=[b_x1f_d], per=f"f{s % 2}")
                    S.dma("sp", lambda e: e.dma_start(out=x1b[t * 128:(t + 1) * 128, :], in_=x1bf[:]), "x1st", reads=[b_x1bf], writes=[b_x1b_d], per="b")

                def tr_stage(_):
                    bk, bb = getbank()
                    bkb = bk[:].bitcast(BF16)
                    for c in range(8):
                        tp(bkb[:, c * 128:(c + 1) * 128], x1bf[:, c * 128:(c + 1) * 128], ident[:], [b_x1bf, b_ident], [bb], c == 7)
                    S.op("dve", lambda e: e.tensor_copy(x1T[:], bkb.rearrange("p (c t) -> p c t", c=8)), reads=[bb], writes=[b_x1T])

                def router_stage(_):
                    bk, bb = getbank()
                    for c in range(8):
                        mm(bk[:, 0:E], x1T[:, c, :], wr[:, c, :], c == 0, c == 7, [b_x1T, b_wr], [bb], c == 7)
                    S.op("dve", lambda e: e.tensor_tensor(out=lg[:, s, :], in0=bk[:, 0:E], in1=br[:], op=ALU.add), reads=[bb, b_br], writes=[b_lg])
                    S.op("dve", lambda e: e.max(out=m8[:, s, :], in_=lg[:, s, :]), reads=[b_lg], writes=[b_m8])

                return [half_stage(0), half_stage(1), rstd_stage, norm_stage, tr_stage, router_stage]

            WO_J = (5, 6, 8, 9, 11, 13)

            def wo_ln(g):
                for s in range(4):
                    for f in wo_stages(g, s):
                        f(None)

            def routing(g):
                def bc(ap3):
                    return ap3.to_broadcast([128, 4, E])
                S.op("dve", lambda e: e.tensor_tensor(out=msk[:], in0=lg[:], in1=bc(m8[:, :, 3:4]), op=ALU.is_ge), reads=[b_lg, b_m8], writes=[b_msk])
                S.op("dve", lambda e: e.tensor_tensor(out=ex[:], in0=lg[:], in1=bc(m8[:, :, 0:1]), op=ALU.subtract), reads=[b_lg, b_m8], writes=[b_ex])
                S.op("act", lambda e: e.activation(ex[:], ex[:], AF.Exp), reads=[b_ex], writes=[b_ex])
                S.op("dve", lambda e: e.tensor_tensor(out=ex[:], in0=ex[:], in1=msk[:], op=ALU.mult), reads=[b_ex, b_msk], writes=[b_ex])
                S.op("dve", lambda e: e.tensor_reduce(out=sm4[:], in_=ex[:], axis=AX.X, op=ALU.add), reads=[b_ex], writes=[b_sm4])
                S.op("dve", lambda e: e.reciprocal(sm4[:], sm4[:]), reads=[b_sm4], writes=[b_sm4])
                S.op("dve", lambda e: e.tensor_tensor(out=gts[:], in0=ex[:], in1=bc(sm4[:].unsqueeze(2)), op=ALU.mult), reads=[b_ex, b_sm4], writes=[b_gts])
                S.op("dve", lambda e: e.tensor_copy(mskb[:], msk[:]), reads=[b_msk], writes=[b_mskb])
                bk, bb = nb()
                for s in range(4):
                    mm(bk[:, s * E:(s + 1) * E], tri[:], mskb[:, s, :], True, True, [b_tri, b_mskb], [bb], False)
                for s in range(4):
                    mm(bk[:, 128 + s * E:128 + (s + 1) * E], ones[:], mskb[:, s, :], True, True, [b_ones, b_mskb], [bb], s == 3)
                for s in range(4):
                    S.op("dve", lambda e, s=s, bk=bk: e.tensor_tensor(out=pos[:, s, :], in0=bk[:, s * E:(s + 1) * E], in1=base[:], op=ALU.add),
                         reads=[bb, b_base], writes=[b_pos])
                    S.op("dve", lambda e, s=s, bk=bk: e.tensor_tensor(out=base[:], in0=bk[:, 128 + s * E:128 + (s + 1) * E], in1=base[:], op=ALU.add),
                         reads=[bb, b_base], writes=[b_base])
                S.op("dve", lambda e: e.tensor_scalar(out=okf[:], in0=pos[:], scalar1=float(C), scalar2=None, op0=ALU.is_lt), reads=[b_pos], writes=[b_okf])
                S.op("dve", lambda e: e.tensor_tensor(out=pos[:], in0=pos[:], in1=ecb[:], op=ALU.add), reads=[b_pos, b_ecb], writes=[b_pos])
                S.op("dve", lambda e: e.tensor_scalar(out=sls[:], in0=pos[:], scalar1=-BIG, scalar2=None, op0=ALU.add), reads=[b_pos], writes=[b_sls])
                S.op("dve", lambda e: e.tensor_tensor(out=sls[:], in0=sls[:], in1=okf[:], op=ALU.mult), reads=[b_sls, b_okf], writes=[b_sls])
                S.op("dve", lambda e: e.tensor_scalar(out=sls[:], in0=sls[:], scalar1=BIG, scalar2=None, op0=ALU.add), reads=[b_sls], writes=[b_sls])
                S.op("dve", lambda e: e.tensor_scalar(out=slg[:], in0=pos[:], scalar1=-float(ZROW), scalar2=None, op0=ALU.add), reads=[b_pos], writes=[b_slg])
                S.op("dve", lambda e: e.tensor_tensor(out=slg[:], in0=slg[:], in1=okf[:], op=ALU.mult), reads=[b_slg, b_okf], writes=[b_slg])
                S.op("dve", lambda e: e.tensor_scalar(out=slg[:], in0=slg[:], scalar1=float(ZROW), scalar2=None, op0=ALU.add), reads=[b_slg], writes=[b_slg])
                for j in range(4):
                    S.op("dve", lambda e, j=j: e.tensor_tensor(out=oh[:], in0=lg[:], in1=bc(m8[:, :, j:j + 1]), op=ALU.is_equal),
                         reads=[b_lg, b_m8], writes=[b_oh])
                    for (src, bsrc, dst, bdst) in ((sls, b_sls, f4s, b_f4s), (slg, b_slg, f4g, b_f4g)):
                        S.op("dve", lambda e, src=src: e.tensor_tensor(out=tmp[:], in0=oh[:], in1=src[:], op=ALU.mult), reads=[b_oh, bsrc], writes=[b_tmp])
                        S.op("dve", lambda e, dst=dst, j=j: e.tensor_reduce(out=dst[:, :, j], in_=tmp[:], axis=AX.X, op=ALU.add), reads=[b_tmp], writes=[bdst])
                    S.op("dve", lambda e: e.tensor_tensor(out=tmp[:], in0=oh[:], in1=gts[:], op=ALU.mult), reads=[b_oh, b_gts], writes=[b_tmp])
                    S.op("dve", lambda e, j=j, g=g: e.tensor_reduce(out=gate4[:, 4 * g:4 * g + 4, j], in_=tmp[:], axis=AX.X, op=ALU.add),
                         reads=[b_tmp], writes=[b_gate4])
                S.op("dve", lambda e, g=g: e.tensor_copy(slot4s[:, 4 * g:4 * g + 4, :], f4s[:]), reads=[b_f4s], writes=[b_slot4s])
                S.op("dve", lambda e, g=g: e.tensor_copy(slot4g[:, 4 * g:4 * g + 4, :], f4g[:]), reads=[b_f4g], writes=[b_slot4g])
                for s in range(4):
                    t = 4 * g + s
                    for j in range(4):
                        S.dma("pool", lambda e, t=t, j=j: e.indirect_dma_start(
                            out=tab, out_offset=bass.IndirectOffsetOnAxis(ap=slot4s[:, t, j:j + 1], axis=0),
                            in_=tokid[:, t, :], in_offset=None, bounds_check=breg, oob_is_err=False),
                            "scat", reads=[b_slot4s, b_tokid, b_tab])

            def finish_counts():
                S.op("dve", lambda e: e.tensor_scalar(out=pos[0:1, 0, :], in0=base[0:1, :], scalar1=-1.0, scalar2=float(SEQ), op0=ALU.mult, op1=ALU.add),
                     reads=[b_base], writes=[b_pos])
                S.op("dve", lambda e: e.tensor_copy(cntv[:], pos[0:1, 0, :]), reads=[b_pos], writes=[b_cntv])

            proj(0)
            attn(0)
            proj(1)
            for g in range(1, NG):
                attn(g, inject=lambda h, g=g: [(jj, f, None) for jj, f in zip(WO_J, wo_stages(g - 1, h, bank=7))])
                if g + 1 < NG:
                    proj(g + 1)
                    if g + 1 == NG - 1:
                        dead = b_win + b_xT
                        for hh in range(2):
                            S.dma("pool", lambda e, hh=hh: e.dma_start(
                                out=wup0[:, :, hh * D:(hh + 1) * D],
                                in_=w_up[0, :, hh * D:(hh + 1) * D].rearrange("(c p) n -> p c n", p=128)),
                                "wld", writes=[b_wup[0][hh]] + dead, per=f"u0{hh}")
                        S.dma("pool", lambda e: e.dma_start(out=wdn0, in_=w_dn[0].rearrange("(c p) n -> p c n", p=128)),
                              "wld", writes=[b_wdn[0]] + dead, per="d0")
                routing(g - 1)
            wo_ln(NG - 1)
            routing(NG - 1)
            finish_counts()
            S.barrier()

        rot["list"] = list(range(8))
        with ExitStack() as p5:
            wup1 = p5.enter_context(nc.sbuf_tensor("wup1", [128, 8, 2 * D], BF16))
            wdn1 = p5.enter_context(nc.sbuf_tensor("wdn1", [128, 8, D], BF16))
            wup = [wup0, wup1[:]]
            wdn = [(wdn0, b_wdn[0]), (wdn1[:], b_wdn[1])]
            bd = [T(p5, f"bd{i}", [1, D], BF16) for i in range(2)]
            idx = [T(p5, f"idx{i}", [128, NB], I32) for i in range(2)]
            xg = [[T(p5, f"xg{i}_{b}", [128, D], BF16) for b in range(NB)] for i in range(1)]
            xgT = [p5.enter_context(nc.sbuf_tensor(f"xgT{i}", [128, 8, C], BF16)) for i in range(2)]
            b_xgT = [[Buf(f"xgT{i}_{b}") for b in range(NB)] for i in range(2)]
            actT = [p5.enter_context(nc.sbuf_tensor(f"actT{i}", [128, 8, C], BF16)) for i in range(2)]
            b_actT = [[[Buf(f"actT{i}_{m}_{gi}") for gi in range(2)] for m in range(8)] for i in range(2)]
            gt = [T(p5, f"gt{i}", [128, 512], F32) for i in range(2)]
            sg = [T(p5, f"sg{i}", [128, 512], F32) for i in range(2)]
            lt = [T(p5, f"lt{i}", [128, 512], F32) for i in range(2)]
            ysb = [T(p5, f"ysb{i}", [128, D], BF16) for i in range(2)]
            b_yall = Buf("yall")

            def load_w(e_, big=True):
                i = e_ % 2
                if big:
                    for hh in range(2):
                        S.dma("pool", lambda e, hh=hh: e.dma_start(
                            out=wup[i][:, :, hh * D:(hh + 1) * D],
                            in_=w_up[e_, :, hh * D:(hh + 1) * D].rearrange("(c p) n -> p c n", p=128)),
                            "wld", writes=[b_wup[i][hh]], per=f"u{i}{hh}")
                    S.dma("pool", lambda e: e.dma_start(out=wdn[i][0], in_=w_dn[e_].rearrange("(c p) n -> p c n", p=128)),
                          "wld", writes=[wdn[i][1]], per=f"d{i}")
                S.dma("pool", lambda e: e.dma_start(out=bd[i][0][:], in_=b_dn[e_:e_ + 1, :]), "wld", writes=[bd[i][1]], per=f"b{i}")

            def load_idx(e_):
                i = e_ % 2
                S.dma("pool", lambda e: e.dma_start(out=idx[i][0][:], in_=tab[e_ * C:(e_ + 1) * C, 0:1].rearrange("(b p) o -> p (b o)", p=128)),
                      "idxld", writes=[idx[i][1]], per=i)

            def load_x(e_):
                i = e_ % 2
                for b in range(NB):
                    S.dma("pool", lambda e, b=b: e.indirect_dma_start(
                        out=xg[0][b][0][:], out_offset=None, in_=x1b,
                        in_offset=bass.IndirectOffsetOnAxis(ap=idx[i][0][:, b:b + 1], axis=0)),
                        "xgat", reads=[idx[i][1]], writes=[xg[0][b][1]], per=b)

            units = [(0, 512, None), (512, 768, 512), (768, 1024, 768)]
            b_actTu = [[[Buf(f"actT{i}_{m}_{u}") for u in range(3)] for m in range(8)] for i in range(2)]
            cregs = {name: eng.alloc_register(f"cnt_{name}") for name, eng in S.engs.items()}
            state = {"k": 0, "y": 0}

            def tr_unit(e_, u):
                i = e_ % 2
                n0, n1, _ = units[u]
                for b in range(n0 // 128, n1 // 128):
                    bk, bb = nb()
                    bkb = bk[:].bitcast(BF16)
                    for c in range(8):
                        tp(bkb[:, c * 128:(c + 1) * 128], xg[0][b][0][:, c * 128:(c + 1) * 128], ident[:],
                           [xg[0][b][1], b_ident], [bb], c == 7)
                    if b % 2 == 0:
                        S.op("act", lambda e, b=b, bkb=bkb: e.activation(xgT[i][:, :, b * 128:(b + 1) * 128], bkb.rearrange("p (c t) -> p c t", c=8), AF.Copy),
                             reads=[bb], writes=[b_xgT[i][b]])
                    else:
                        S.op("dve", lambda e, b=b, bkb=bkb: e.tensor_copy(xgT[i][:, :, b * 128:(b + 1) * 128], bkb.rearrange("p (c t) -> p c t", c=8)),
                             reads=[bb], writes=[b_xgT[i][b]])

            def work_unit(e_, u):
                i = e_ % 2
                n0, n1, _ = units[u]
                n = n1 - n0
                rb = [b_xgT[i][b] for b in range(n0 // 128, n1 // 128)]
                for mp in range(8):
                    bkg, bbg = nb()
                    for c in range(8):
                        mm(bkg[:, 0:n], wup[i][:, c, mp * 128:(mp + 1) * 128], xgT[i][:, c, n0:n1], c == 0, c == 7,
                           [b_wup[i][0]] + rb, [bbg], c == 7)
                    bkl, bbl = nb()
                    for c in range(8):
                        mm(bkl[:, 0:n], wup[i][:, c, D + mp * 128:D + (mp + 1) * 128], xgT[i][:, c, n0:n1], c == 0, c == 7,
                           [b_wup[i][1]] + rb, [bbl], c == 7)
                    gtt, b_gtt = gt[state["k"] % 2]
                    sgt, b_sgt = sg[state["k"] % 2]
                    ltt, b_ltt = lt[state["k"] % 2]
                    state["k"] += 1
                    S.op("dve", lambda e, gtt=gtt, bkg=bkg, mp=mp: e.tensor_scalar(out=gtt[:, 0:n], in0=bkg[:, 0:n], scalar1=bupT[:, mp, e_:e_ + 1], scalar2=7.0,
                                                          op0=ALU.add, op1=ALU.min), reads=[bbg, b_bupT], writes=[b_gtt])
                    S.op("act", lambda e, ltt=ltt, bkl=bkl, mp=mp: e.activation(ltt[:, 0:n], bkl[:, 0:n], AF.Identity, bias=bupT[:, 8 + mp, e_:e_ + 1],
                                                                               scale=1.0 / 1.702),
                         reads=[bbl, b_bupT], writes=[b_ltt])
                    S.op("act", lambda e, sgt=sgt, gtt=gtt: e.activation(sgt[:, 0:n], gtt[:, 0:n], AF.Silu, scale=1.702), reads=[b_gtt], writes=[b_sgt])
                    S.op("dve", lambda e, ltt=ltt: e.tensor_scalar(out=ltt[:, 0:n], in0=ltt[:, 0:n], scalar1=7.0 / 1.702, scalar2=-7.0 / 1.702,
                                                                  op0=ALU.min, op1=ALU.max),
                         reads=[b_ltt], writes=[b_ltt])
                    S.op("dve", lambda e, ltt=ltt, sgt=sgt, mp=mp: e.scalar_tensor_tensor(out=actT[i][:, mp, n0:n1], in0=ltt[:, 0:n], scalar=1.0 / 1.702,
                                                                                         in1=sgt[:, 0:n], op0=ALU.add, op1=ALU.mult),
                         reads=[b_ltt, b_sgt], writes=[b_actTu[i][mp][u]])
                for b in range(n0 // 128, n1 // 128):
                    yi = state["y"] % 2
                    state["y"] += 1
                    yt, b_yt = ysb[yi]
                    for half in range(2):
                        bk, bb = nb()
                        mm(bk[:, :], ones[0:1, :], bd[i][0][0:1, half * 512:(half + 1) * 512], True, False, [b_ones, bd[i][1]], [bb], False)
                        for m in range(8):
                            mm(bk[:, :], actT[i][:, m, b * 128:(b + 1) * 128], wdn[i][0][:, m, half * 512:(half + 1) * 512], False, m == 7,
                               [b_actTu[i][m][u], wdn[i][1]], [bb], m == 7)
                        if half == 0:
                            S.op("act", lambda e, yt=yt, bk=bk: e.activation(yt[:, 0:512], bk[:, :], AF.Copy), reads=[bb], writes=[b_yt])
                        else:
                            S.op("dve", lambda e, yt=yt, bk=bk: e.tensor_copy(yt[:, 512:1024], bk[:, :]), reads=[bb], writes=[b_yt])
                    r0 = e_ * C + b * 128
                    S.dma("sp", lambda e, r0=r0, yt=yt: e.dma_start(out=y_all[r0:r0 + 128, :], in_=yt[:]), "yst", reads=[b_yt], writes=[b_yall], per=yi)

            with nc.allow_non_contiguous_dma(reason="small per-expert slot index table"):
                load_idx(0)
                load_x(0)
                load_w(0, big=False)
                for e_ in range(E):
                    for name, eng in S.engs.items():
                        eng.reg_load(cregs[name], cntv[0:1, e_:e_ + 1])
                    if e_ + 1 < E:
                        load_idx(e_ + 1)
                    rot["list"], rot["p"] = [0, 1, 2, 3], 0
                    tr_unit(e_, 0)
                    for u in (1, 2):
                        rot["list"], rot["p"] = [4, 5, 6, 7], 0
                        S.cond_region(cregs, SEQ - units[u][2], lambda u=u: tr_unit(e_, u))
                    rot["list"], rot["p"] = [0, 1, 2, 3], 0
                    if e_ + 1 < E:
                        load_x(e_ + 1)
                        load_w(e_ + 1)
                    work_unit(e_, 0)
                    for u in (1, 2):
                        rot["list"], rot["p"] = [4, 5, 6, 7], 0
                        S.cond_region(cregs, SEQ - units[u][2], lambda u=u: work_unit(e_, u))
                    rot["list"], rot["p"] = [0, 1, 2, 3], 0
            S.barrier()

        with ExitStack() as p6:
            yg = [[T(p6, f"yg{i}_{j}", [128, D], BF16) for j in range(4)] for i in range(2)]
            accb = [T(p6, f"acc{i}", [128, D], F32) for i in range(2)]
            xr = [T(p6, f"xr{i}", [128, D], F32) for i in range(2)]
            st3 = [T(p6, f"st3_{i}", [128, 2, 6], F32) for i in range(2)]
            mv3 = [T(p6, f"mv3_{i}", [128, 2], F32) for i in range(2)]
            rs3 = [T(p6, f"rs3_{i}", [128, 1], F32) for i in range(2)]
            ln2g, b_ln2g = T(p6, "ln2g", [128, D], F32)
            ln2b, b_ln2b = T(p6, "ln2b", [128, D], F32)
            bcast_load(ln2g[:], ln2_g, D, b_ln2g)
            bcast_load(ln2b[:], ln2_b, D, b_ln2b)
            last_tok = None

            def gathers(t):
                i = t % 2
                for j in range(4):
                    S.dma("pool", lambda e, j=j: e.indirect_dma_start(
                        out=yg[i][j][0][:], out_offset=None, in_=y_all,
                        in_offset=bass.IndirectOffsetOnAxis(ap=slot4g[:, t, j:j + 1], axis=0)),
                        "ygat", reads=[b_slot4g], writes=[yg[i][j][1]], per=f"{i}{j}")
                S.dma("sp", lambda e: e.dma_start(out=xr[i][0][:], in_=x1f[t * 128:(t + 1) * 128, :]), "xrld", writes=[xr[i][1]], per=i)

            gathers(0)
            for t in range(NT):
                i = t % 2
                if t + 1 < NT:
                    gathers(t + 1)
                xrt, b_xrt = xr[i]
                act_, b_act = accb[i]
                S.op("act", lambda e: e.activation(act_[:], yg[i][0][0][:], AF.Copy, scale=gate4[:, t, 0:1]),
                     reads=[yg[i][0][1], b_gate4], writes=[b_act])
                for j in range(1, 4):
                    S.op("dve", lambda e, j=j: e.scalar_tensor_tensor(out=act_[:], in0=yg[i][j][0][:], scalar=gate4[:, t, j:j + 1], in1=act_[:],
                                                                       op0=ALU.mult, op1=ALU.add),
                         reads=[yg[i][j][1], b_gate4, b_act], writes=[b_act])
                S.op("dve", lambda e: e.scalar_tensor_tensor(out=act_[:], in0=xrt[:], scalar=ALPHA, in1=act_[:], op0=ALU.mult, op1=ALU.add),
                     reads=[b_xrt, b_act], writes=[b_act])
                stt, b_stt = st3[i]
                mvt, b_mvt = mv3[i]
                rst, b_rst = rs3[i]
                for half in range(2):
                    S.op("dve", lambda e, half=half: e.bn_stats(stt[:, half, :], act_[:, half * 512:(half + 1) * 512]), reads=[b_act], writes=[b_stt])
                S.op("dve", lambda e: e.bn_aggr(mvt[:], stt[:]), reads=[b_stt], writes=[b_mvt])
                S.op("act", lambda e: e.activation(rst[:], mvt[:, 1:2], AF.Ln, bias=EPS, scale=1.0), reads=[b_mvt], writes=[b_rst])
                S.op("act", lambda e: e.activation(rst[:], rst[:], AF.Exp, scale=-0.5), reads=[b_rst], writes=[b_rst])
                S.op("dve", lambda e: e.scalar_tensor_tensor(out=mvt[:, 0:1], in0=mvt[:, 0:1], scalar=-1.0, in1=rst[:, 0:1], op0=ALU.mult, op1=ALU.mult),
                     reads=[b_mvt, b_rst], writes=[b_mvt])
                S.op("act", lambda e: e.activation(act_[:], act_[:], AF.Identity, bias=mvt[:, 0:1], scale=rst[:, 0:1]),
                     reads=[b_act, b_mvt, b_rst], writes=[b_act])
                S.op("dve", lambda e: e.tensor_tensor(out=act_[:], in0=act_[:], in1=ln2g[:], op=ALU.mult), reads=[b_act, b_ln2g], writes=[b_act])
                S.op("pool", lambda e: e.tensor_tensor(out=act_[:], in0=act_[:], in1=ln2b[:], op=ALU.add), reads=[b_act, b_ln2b], writes=[b_act])
                last_tok = S.dma("sp", lambda e: e.dma_start(out=out[t * 128:(t + 1) * 128, :], in_=act_[:]), "ost", reads=[b_act], per=i)
            S.barrier()
    return nc


_CACHE = {}


def _consts():
    p = np.arange(128)
    c = {}
    c["c_ident"] = np.eye(128, dtype=np.float32)
    c["c_maskT"] = (p[:, None] <= p[None, :]).astype(np.float32)
    c["c_tri"] = (p[:, None] < p[None, :]).astype(np.float32)
    c["c_ones"] = np.ones((128, 128), np.float32)
    c["c_tril"] = (p[None, :] <= p[:, None]).astype(np.float32)
    c["c_tokid"] = np.broadcast_to((np.arange(NT)[None, :] * 128 + p[:, None]).astype(np.int32)[:, :, None], (128, NT, TABW)).copy()
    c["c_ecb"] = np.broadcast_to((np.arange(E) * C).astype(np.float32)[None, None, :], (128, 4, E)).copy()
    return c


def kernel(**inputs):
    if "nc" not in _CACHE:
        _CACHE["nc"] = build()
    nc = _CACHE["nc"]
    f = lambda a: np.ascontiguousarray(np.asarray(a, dtype=np.float32))
    shared = {
        "w_in": f(inputs["w_in"][0]),
        "lambda_q1": f(inputs["lambda_q1"]), "lambda_k1": f(inputs["lambda_k1"]),
        "lambda_q2": f(inputs["lambda_q2"]), "lambda_k2": f(inputs["lambda_k2"]),
        "subln_g": f(inputs["subln_g"]),
        "gmlp_ln_g": f(inputs["gmlp_ln_g"]), "gmlp_ln_b": f(inputs["gmlp_ln_b"]),
        "w_spatial": f(inputs["w_spatial"][0]), "b_spatial": f(inputs["b_spatial"][0]),
        "w_o": f(inputs["w_o"][0]),
        "ln1_g": f(inputs["ln1_g"]), "ln1_b": f(inputs["ln1_b"]),
        "w_router": f(inputs["w_router"][0]), "b_router": f(inputs["b_router"]),
        "w_up": f(inputs["w_up"][0]), "b_up": f(inputs["b_up"][0]),
        "w_down": f(inputs["w_down"][0]), "b_down": f(inputs["b_down"][0]),
        "ln2_g": f(inputs["ln2_g"]), "ln2_b": f(inputs["ln2_b"]),
    }
    shared.update(_consts())
    xs = np.asarray(inputs["x"], dtype=np.float32)
    in_maps = [dict(shared, x=np.ascontiguousarray(xs[b])) for b in range(N_CORES)]
    res = run_bass_kernel_spmd(nc, in_maps, core_ids=list(range(N_CORES)))
    return np.stack([np.asarray(r["out"], dtype=np.float32) for r in res.results], axis=0)
```
